# Optimizing a Trainium2 kernel written in Bass

```python
import jax, jax.numpy as jnp
from jax import lax
import numpy as np

D_MODEL = 1024
BATCH = 4
SEQ = 4096
DEPTH = 1

N_MEM = 256
ATTN_HEADS = 8
ATTN_HEAD_DIM = D_MODEL // ATTN_HEADS
ATTN_WIDTH = ATTN_HEADS * ATTN_HEAD_DIM
MOBA_BLOCK = 256
MOBA_TOPK = 3
Q_CHUNK = 128
CONV_CH = D_MODEL
CONV_WIDTH = 31
XATTN_HEADS = 4
XATTN_HEAD_DIM = D_MODEL // XATTN_HEADS
PEER_HEADS = 8
PEER_QDIM = 256
PEER_NKEYS = 128
PEER_EXPERTS = PEER_NKEYS * PEER_NKEYS
PEER_TOPK = 16
PEER_CHUNK = 128
IN_WIDTH = 3 * ATTN_WIDTH + 2 * CONV_CH + 2 * D_MODEL
EPS = 1e-6

kernel_name = 'hybrid_conformer_moba_peer_block'


def rms_norm(x, g):
    xf = x.astype(jnp.float32)
    y = xf * lax.rsqrt(jnp.mean(xf * xf, axis=-1, keepdims=True) + EPS) * g.astype(jnp.float32)
    return y.astype(x.dtype)


def layer_norm(x, g, b):
    xf = x.astype(jnp.float32)
    mu = jnp.mean(xf, axis=-1, keepdims=True)
    var = jnp.mean(jnp.square(xf - mu), axis=-1, keepdims=True)
    y = (xf - mu) * lax.rsqrt(var + EPS) * g.astype(jnp.float32) + b.astype(jnp.float32)
    return y.astype(x.dtype)


def alibi_slopes(n):
    return 2.0 ** (-8.0 * jnp.arange(1, n + 1, dtype=jnp.float32) / n)


def split_heads(t, n_heads):
    B, S, W = t.shape
    return t.reshape(B, S, n_heads, W // n_heads).transpose(0, 2, 1, 3)


def conformer_conv(a, b, dw_w, dw_b, ln_g, ln_b, w_out, b_out):
    u = a * jax.nn.sigmoid(b)
    C = u.shape[-1]
    u = lax.conv_general_dilated(u, dw_w[:, None, :].astype(u.dtype), window_strides=(1,),
                                 padding=[(CONV_WIDTH - 1, 0)],
                                 dimension_numbers=('NWC', 'WIO', 'NWC'),
                                 feature_group_count=C) + dw_b
    u = jax.nn.silu(layer_norm(u, ln_g, ln_b))
    return u @ w_out + b_out


def moba_attention(q, k, v):
    B, H, S, hd = q.shape
    L = MOBA_BLOCK
    nb = -(-S // L)
    pad = nb * L - S
    if pad:
        k = jnp.pad(k, ((0, 0), (0, 0), (0, pad), (0, 0)))
        v = jnp.pad(v, ((0, 0), (0, 0), (0, pad), (0, 0)))
    k_blocks = k.reshape(B, H, nb, L, hd)
    v_blocks = v.reshape(B, H, nb, L, hd)
    k_mean = jnp.mean(k_blocks.astype(jnp.float32), axis=3)
    n_sel = min(MOBA_TOPK, nb)
    slopes = alibi_slopes(H)[None, :, None, None]
    scale = hd ** -0.5
    b_idx = jnp.arange(B)[:, None, None]
    h_idx = jnp.arange(H)[None, :, None]
    offs = jnp.arange(L)

    def chunk(c):
        start = c * Q_CHUNK
        blk = start // L
        qc = lax.dynamic_slice_in_dim(q, start, Q_CHUNK, axis=2)
        t = (start + jnp.arange(Q_CHUNK))[None, None, :, None]
        gate = jnp.einsum('bhqd,bhnd->bhqn', qc.astype(jnp.float32), k_mean)
        gate = jnp.where(jnp.arange(nb) < blk, gate, -jnp.inf)
        _, sel = lax.top_k(gate, n_sel)
        logits = []
        for j in range(n_sel):
            idx = sel[..., j]
            kj = k_blocks[b_idx, h_idx, idx]
            s = jnp.einsum('bhqd,bhqld->bhql', qc, kj).astype(jnp.float32) * scale
            pos = idx[..., None] * L + offs
            s = s - slopes * (t - pos).astype(jnp.float32)
            logits.append(jnp.where(j < blk, s, -jnp.inf))
        k_own = lax.dynamic_slice_in_dim(k, blk * L, L, axis=2)
        v_own = lax.dynamic_slice_in_dim(v, blk * L, L, axis=2)
        s = jnp.einsum('bhqd,bhld->bhql', qc, k_own).astype(jnp.float32) * scale
        pos = blk * L + offs
        s = s - slopes * (t - pos).astype(jnp.float32)
        logits.append(jnp.where(pos <= t, s, -jnp.inf))
        p = jax.nn.softmax(jnp.concatenate(logits, axis=-1), axis=-1).astype(v.dtype)
        out = jnp.einsum('bhql,bhld->bhqd', p[..., n_sel * L:], v_own)
        for j in range(n_sel):
            vj = v_blocks[b_idx, h_idx, sel[..., j]]
            out = out + jnp.einsum('bhql,bhqld->bhqd', p[..., j * L:(j + 1) * L], vj)
        return out

    out = lax.map(chunk, jnp.arange(S // Q_CHUNK))
    return out.transpose(1, 0, 3, 2, 4).reshape(B, S, H * hd)


def memory_cross_attention(hn, mn, w_q, w_kv, w_o):
    B, S, D = hn.shape
    M = mn.shape[1]
    q = (hn @ w_q).reshape(B, S, XATTN_HEADS, XATTN_HEAD_DIM)
    kv = (mn @ w_kv).reshape(B, M, 2, XATTN_HEADS, XATTN_HEAD_DIM)
    k, v = kv[:, :, 0], kv[:, :, 1]
    s = jnp.einsum('bshd,bmhd->bhsm', q, k).astype(jnp.float32) * (XATTN_HEAD_DIM ** -0.5)
    p = jax.nn.softmax(s, axis=-1).astype(v.dtype)
    o = jnp.einsum('bhsm,bmhd->bshd', p, v).reshape(B, S, D)
    return o @ w_o


def peer_ffn(xn, w_q, subkeys, u_tab, v_tab):
    B, S, D = xn.shape
    nc = S // PEER_CHUNK
    half = PEER_QDIM // 2
    xc = xn.reshape(B, nc, PEER_CHUNK, D).transpose(1, 0, 2, 3)

    def chunk(xb):
        q = (xb @ w_q).reshape(B, PEER_CHUNK, PEER_HEADS, PEER_QDIM)
        s1 = jnp.einsum('bqhd,hnd->bqhn', q[..., :half], subkeys[:, 0]).astype(jnp.float32)
        s2 = jnp.einsum('bqhd,hnd->bqhn', q[..., half:], subkeys[:, 1]).astype(jnp.float32)
        v1, i1 = lax.top_k(s1, PEER_TOPK)
        v2, i2 = lax.top_k(s2, PEER_TOPK)
        cand = (v1[..., :, None] + v2[..., None, :]).reshape(B, PEER_CHUNK, PEER_HEADS, PEER_TOPK * PEER_TOPK)
        sc, ci = lax.top_k(cand, PEER_TOPK)
        e = (jnp.take_along_axis(i1, ci // PEER_TOPK, axis=-1) * PEER_NKEYS
             + jnp.take_along_axis(i2, ci % PEER_TOPK, axis=-1))
        g = jax.nn.softmax(sc, axis=-1)
        ue = u_tab[e]
        a = jax.nn.gelu(jnp.einsum('bqhkd,bqd->bqhk', ue, xb).astype(jnp.float32), approximate=False)
        ve = v_tab[e]
        return jnp.einsum('bqhk,bqhkd->bqd', (g * a).astype(ve.dtype), ve)

    out = lax.map(chunk, xc)
    return out.transpose(1, 0, 2, 3).reshape(B, S, D)


def setup_inputs(seed: int = 0) -> dict:
    key = jax.random.key(seed)
    ks = jax.random.split(key, 24)
    L = DEPTH

    def nrm(k, shape, scale):
        return jax.random.normal(k, shape, jnp.float32) * scale

    def gain(k, shape):
        return 1.0 + 0.05 * jax.random.normal(k, shape, jnp.float32)

    return {
        'x': nrm(ks[0], (BATCH, SEQ, D_MODEL), 1.0),
        'mem': nrm(ks[1], (BATCH, N_MEM, D_MODEL), 1.0),
        'mix_norm_g': gain(ks[2], (L, D_MODEL)),
        'w_in': nrm(ks[3], (L, D_MODEL, IN_WIDTH), D_MODEL ** -0.5),
        'conv_dw_w': nrm(ks[4], (L, CONV_WIDTH, CONV_CH), CONV_WIDTH ** -0.5),
        'conv_dw_b': nrm(ks[5], (L, CONV_CH), 0.02),
        'conv_ln_g': gain(ks[6], (L, CONV_CH)),
        'conv_ln_b': nrm(ks[7], (L, CONV_CH), 0.02),
        'w_conv_out': nrm(ks[8], (L, CONV_CH, D_MODEL), CONV_CH ** -0.5),
        'b_conv_out': nrm(ks[9], (L, D_MODEL), 0.02),
        'w_attn_out': nrm(ks[10], (L, ATTN_WIDTH, D_MODEL), ATTN_WIDTH ** -0.5),
        'w_mix_out': nrm(ks[11], (L, D_MODEL, D_MODEL), D_MODEL ** -0.5),
        'xattn_norm_g': gain(ks[12], (L, D_MODEL)),
        'mem_norm_g': gain(ks[13], (L, D_MODEL)),
        'w_xq': nrm(ks[14], (L, D_MODEL, D_MODEL), D_MODEL ** -0.5),
        'w_xkv': nrm(ks[15], (L, D_MODEL, 2 * D_MODEL), D_MODEL ** -0.5),
        'w_xo': nrm(ks[16], (L, D_MODEL, D_MODEL), D_MODEL ** -0.5),
        'ffn_norm_g': gain(ks[17], (L, D_MODEL)),
        'w_peer_q': nrm(ks[18], (L, D_MODEL, PEER_HEADS * PEER_QDIM), D_MODEL ** -0.5),
        'peer_subkeys': nrm(ks[19], (L, PEER_HEADS, 2, PEER_NKEYS, PEER_QDIM // 2), (PEER_QDIM // 2) ** -0.5),
        'peer_u': nrm(ks[20], (L, PEER_EXPERTS, D_MODEL), D_MODEL ** -0.5),
        'peer_v': nrm(ks[21], (L, PEER_EXPERTS, D_MODEL), PEER_HEADS ** -0.5),
        'final_norm_g': gain(ks[22], (D_MODEL,)),
    }


def reference(x, mem, mix_norm_g, w_in, conv_dw_w, conv_dw_b, conv_ln_g, conv_ln_b, w_conv_out, b_conv_out,
              w_attn_out, w_mix_out, xattn_norm_g, mem_norm_g, w_xq, w_xkv, w_xo, ffn_norm_g, w_peer_q,
              peer_subkeys, peer_u, peer_v, final_norm_g):
    A, C, D = ATTN_WIDTH, CONV_CH, D_MODEL
    cuts = [A, 2 * A, 3 * A, 3 * A + C, 3 * A + 2 * C, 3 * A + 2 * C + D]
    h = x
    for l in range(DEPTH):
        xn = rms_norm(h, mix_norm_g[l])
        proj = xn @ w_in[l]
        q, k, v, glu_a, glu_b, gate_c, gate_a = jnp.split(proj, cuts, axis=-1)
        conv_out = conformer_conv(glu_a, glu_b, conv_dw_w[l], conv_dw_b[l], conv_ln_g[l], conv_ln_b[l],
                                  w_conv_out[l], b_conv_out[l])
        attn = moba_attention(split_heads(q, ATTN_HEADS), split_heads(k, ATTN_HEADS),
                              split_heads(v, ATTN_HEADS)) @ w_attn_out[l]
        merged = jax.nn.sigmoid(gate_c) * conv_out + jax.nn.sigmoid(gate_a) * attn
        h = h + merged @ w_mix_out[l]
        h = h + memory_cross_attention(rms_norm(h, xattn_norm_g[l]), rms_norm(mem, mem_norm_g[l]),
                                       w_xq[l], w_xkv[l], w_xo[l])
        h = h + peer_ffn(rms_norm(h, ffn_norm_g[l]), w_peer_q[l], peer_subkeys[l], peer_u[l], peer_v[l])
    return rms_norm(h, final_norm_g)
```

```python
import contextlib
import numpy as np
import ml_dtypes
import concourse.bass as bass
import concourse.mybir as mybir
from concourse.bass_utils import run_bass_kernel_spmd

F32 = mybir.dt.float32
BF16 = mybir.dt.bfloat16
U32 = mybir.dt.uint32
I32 = mybir.dt.int32
ALU = mybir.AluOpType
AF = mybir.ActivationFunctionType
AX = mybir.AxisListType

D = 1024
SEQ = 4096
NB_TOK = 2048
EPS = 1e-6
NEG = -30000.0


class Trk:
    __slots__ = ("w", "r")

    def __init__(self):
        self.w = {}
        self.r = {}


class T:
    def __init__(self, t):
        self.t = t
        self.trk = Trk()
        self.sub = {}

    def __getitem__(self, k):
        return self.t[k]

    def k(self, key):
        if key not in self.sub:
            self.sub[key] = Trk()
        return self.sub[key]


class Eng:
    def __init__(self, name, eng, sem):
        self.name = name
        self.eng = eng
        self.sem = sem
        self.count = 0
        self.seen = {}


def _trk(x):
    return x.trk if isinstance(x, T) else x


class KB:
    def __init__(self, nc, es, nds=48):
        self.nc = nc
        self.es = es
        self.pe = Eng("pe", nc.tensor, es.enter_context(nc.semaphore("s_pe")))
        self.act = Eng("act", nc.scalar, es.enter_context(nc.semaphore("s_act")))
        self.dve = Eng("dve", nc.vector, es.enter_context(nc.semaphore("s_dve")))
        self.pool = Eng("pool", nc.gpsimd, es.enter_context(nc.semaphore("s_pool")))
        self.sp = Eng("sp", nc.sync, es.enter_context(nc.semaphore("s_sp")))
        self.dsems = [es.enter_context(nc.semaphore(f"s_d{i}")) for i in range(nds)]
        self.dvals = [0] * nds
        self.dnext = {"hw": 0, "sw": nds // 2}
        self.uid = 0
        self.defer = None

    def name(self, p):
        self.uid += 1
        return f"{p}_{self.uid}"

    def sb(self, shape, dt, es=None, name="sb"):
        return T((es or self.es).enter_context(self.nc.sbuf_tensor(self.name(name), list(shape), dt)))

    def ps(self, shape, dt, es=None, name="ps"):
        return T((es or self.es).enter_context(self.nc.psum_tensor(self.name(name), list(shape), dt)))

    def _deps(self, E, r, w):
        deps = {}
        for x in r:
            for sid, (sem, val) in _trk(x).w.items():
                if deps.get(sid, (None, 0))[1] < val:
                    deps[sid] = (sem, val)
        for x in w:
            tk = _trk(x)
            for dct in (tk.w, tk.r):
                for sid, (sem, val) in dct.items():
                    if deps.get(sid, (None, 0))[1] < val:
                        deps[sid] = (sem, val)
        for sid, (sem, val) in deps.items():
            if E.seen.get(sid, 0) < val:
                if sid == id(E.sem) and E.name in NO_SELF_WAIT:
                    continue
                E.eng.wait_ge(sem, val)
                E.seen[sid] = val

    def _post(self, ev, r, w):
        sid = id(ev[0])
        for x in r:
            tk = _trk(x)
            if tk.r.get(sid, (None, 0))[1] < ev[1]:
                tk.r[sid] = ev
        for x in w:
            tk = _trk(x)
            tk.w = {sid: ev}
            tk.r = {}

    def flush(self, th, n=None):
        k = 0
        while th and (n is None or k < n):
            kind, E, fn, r, w = th.pop(0)
            (self.op if kind == "op" else self.dma)(E, fn, r, w)
            k += 1

    def op(self, E, fn, r=(), w=()):
        if self.defer is not None:
            self.defer.append(("op", E, fn, list(r), list(w)))
            return None
        self._deps(E, r, w)
        ins = fn()
        E.count += 1
        ins.then_inc(E.sem, 1)
        self._post((E.sem, E.count), r, w)
        return ins

    def dma(self, Q, fn, r=(), w=()):
        if self.defer is not None:
            self.defer.append(("dma", Q, fn, list(r), list(w)))
            return None
        self._deps(Q, r, w)
        half = len(self.dsems) // 2
        kq = "sw" if Q.name == "pool" else "hw"
        j = self.dnext[kq]
        base = half if kq == "sw" else 0
        self.dnext[kq] = base + (j - base + 1) % half
        sem = self.dsems[j]
        if self.dvals[j] > 0 and Q.seen.get(id(sem), 0) < self.dvals[j]:
            Q.eng.wait_ge(sem, self.dvals[j])
            Q.seen[id(sem)] = self.dvals[j]
        ins = fn()
        self.dvals[j] += 16
        ins.then_inc(sem, 16)
        self._post((sem, self.dvals[j]), r, w)
        return ins

    def barrier(self):
        engs = (self.pe, self.act, self.dve, self.pool, self.sp)
        for E in engs:
            for F in engs:
                if F is not E and F.count > 0 and E.seen.get(id(F.sem), 0) < F.count:
                    E.eng.wait_ge(F.sem, F.count)
                    E.seen[id(F.sem)] = F.count
            for j, sem in enumerate(self.dsems):
                if self.dvals[j] > 0 and E.seen.get(id(sem), 0) < self.dvals[j]:
                    E.eng.wait_ge(sem, self.dvals[j])
                    E.seen[id(sem)] = self.dvals[j]

    def finish(self, trks):
        self._deps(self.sp, trks, ())
        for j, sem in enumerate(self.dsems):
            if self.dvals[j] > 0 and self.sp.seen.get(id(sem), 0) < self.dvals[j]:
                self.sp.eng.wait_ge(sem, self.dvals[j])
        for E in (self.pe, self.act, self.dve, self.pool):
            if E.count > 0:
                self.sp.eng.wait_ge(E.sem, E.count)


def AP(base, off, dims):
    return bass.AP(tensor=base.tensor, offset=off, ap=[list(d) for d in dims])


def build_program():
    nc = bass.Bass("TRN2", target_bir_lowering=False)

    def din(name, shape, dt=F32):
        return nc.dram_tensor(name, list(shape), dt, kind="ExternalInput").ap()

    io = {}
    io["xfull"] = din("xfull", [SEQ, D])
    io["xown"] = din("xown", [NB_TOK, D])
    io["xconv"] = din("xconv", [8, 288, D])
    io["mem"] = din("mem", [256, D])
    for nm, shp in [("mix_norm_g", [D]), ("w_in", [D, 7 * D]), ("conv_dw_w", [31, D]), ("conv_dw_b", [D]),
                    ("conv_ln_g", [D]), ("conv_ln_b", [D]), ("w_conv_out", [D, D]), ("b_conv_out", [D]),
                    ("w_attn_out", [D, D]), ("w_mix_out", [D, D]), ("xattn_norm_g", [D]), ("mem_norm_g", [D]),
                    ("w_xq", [D, D]), ("w_xkv", [D, 2 * D]), ("w_xo", [D, D]), ("ffn_norm_g", [D]),
                    ("w_peer_q", [D, 2 * D]), ("peer_subkeys", [16, 128, 128]), ("peer_u", [16384, D]),
                    ("peer_v", [16384, D]), ("final_norm_g", [D])]:
        io[nm] = din(nm, shp)
    io["abias"] = din("abias", [16, 128, 128])
    io["vpen"] = din("vpen", [16, 128, 16])
    io["notown"] = din("notown", [16, 128, 16])
    io["cpa"] = din("cpa", [2, 128, 256])
    io["cpb"] = din("cpb", [2, 128, 256])
    io["krow"] = din("krow", [1, 8 * 512])
    io["identf"] = din("identf", [128, 128])
    io["iota16"] = din("iota16", [128, 16])
    out = nc.dram_tensor("out", [NB_TOK, D], F32, kind="ExternalOutput").ap()

    def dscr(name, shape, dt):
        return nc.dram_tensor(name, list(shape), dt, kind=DBG_KIND.get(name, "Internal")).ap()

    kT_d = dscr("kT_d", [8, 128, SEQ], BF16)
    v_d = dscr("v_d", [SEQ, D], BF16)
    convo_d = dscr("convo_d", [D, NB_TOK], BF16)
    h1_d = dscr("h1_d", [NB_TOK, D], F32)
    h2_d = dscr("h2_d", [NB_TOK, D], F32)
    uv_d = dscr("uv_d", [16384, 2 * D], BF16)
    trk_ub = [Trk() for _ in range(8)]
    trk_vb = [Trk() for _ in range(8)]
    trk_kT = [Trk() for _ in range(8)]
    trk_v = [Trk() for _ in range(8)]
    trk_convo = [Trk() for _ in range(8)]
    trk_h1 = [Trk() for _ in range(16)]
    trk_h2 = [Trk() for _ in range(16)]
    trk_out = Trk()

    es = contextlib.ExitStack()
    with es:
        kb = KB(nc, es)
        pe, act, dve, pool, sp = kb.pe, kb.act, kb.dve, kb.pool, kb.sp
        dbg_trks = []

        def dbg(name, tile, shape, dt):
            if not DEBUG:
                return
            d = nc.dram_tensor("dbg_" + name, list(shape), dt, kind="ExternalOutput").ap()
            tk = Trk()
            src = tile if isinstance(tile, T) else tile[0]
            ap = tile[:] if isinstance(tile, T) else tile[1]
            kb.dma(sp, lambda: nc.sync.dma_start(out=d, in_=ap), r=[src], w=[tk])
            dbg_trks.append(tk)

        identf = kb.sb([128, 128], F32, name="identf")
        identb = kb.sb([128, 128], BF16, name="identb")
        kb.dma(sp, lambda: nc.sync.dma_start(out=identf[:], in_=io["identf"]), w=[identf])
        kb.dma(pool, lambda: nc.gpsimd.dma_start(out=identb[:], in_=io["identf"]), w=[identb])
        kmeanT = kb.sb([128, 8, 16], BF16, name="kmeanT")
        epsc = kb.sb([128, 1], F32, name="epsc")
        kb.op(dve, lambda: nc.vector.memset(epsc[:], EPS), w=[epsc])
        def load_w(dst, src_ap, cols, q=None):
            src = src_ap.rearrange("(dc p) n -> p dc n", p=128)[:, :, cols[0]:cols[1]]
            for dc in range(0, 8, 2):
                kb.dma(pool, (lambda dc=dc: nc.gpsimd.dma_start(out=dst[:, dc:dc + 2, :], in_=src[:, dc:dc + 2, :])),
                       w=[dst])

        def colvec(src_ap, es_, n=8):
            t = kb.sb([128, n], F32, es=es_, name="colv")
            kb.dma(sp, lambda: nc.sync.dma_start(out=t[:], in_=src_ap.rearrange("(c p) -> p c", p=128),
                                                 allow_slow_non_contiguous=True), w=[t])
            return t

        class NormT:
            def __init__(self, es_, nbuf=2, npsum=None):
                self.junk = [kb.sb([128, D], BF16, es=es_, name="junk") for _ in range(nbuf)]
                self.ss = [kb.sb([128, 1], F32, es=es_, name="ss") for _ in range(nbuf)]
                self.rs = [kb.sb([128, 1], F32, es=es_, name="rs") for _ in range(nbuf)]
                self.xnb = [kb.sb([128, D], BF16, es=es_, name="xnb") for _ in range(nbuf)]
                self.pT = [kb.ps([128, D], BF16, es=es_, name="pT") for _ in range(npsum or nbuf)]
                self.i = 0
                self.nbuf = nbuf
                self.es_ = es_
                self.g = {}

            def getg(self, nm):
                if nm not in self.g:
                    g = kb.sb([128, D], F32, es=self.es_, name="grep")
                    src = AP(io[nm], 0, [[0, 128], [1, D]])
                    kb.dma(sp, (lambda g=g, src=src: nc.sync.dma_start(out=g[:], in_=src)), w=[g])
                    self.g[nm] = g
                return self.g[nm]

            def run(self, xt, P, gname, dstT, col0, xn_f32=None, dst_trk=None):
                b = self.i % self.nbuf
                self.i += 1
                junk, ss, rs, xnb, pT = self.junk[b], self.ss[b], self.rs[b], self.xnb[b], self.pT[b % len(self.pT)]
                g = self.getg(gname)
                kb.op(act, lambda: nc.scalar.activation(out=junk[0:P, :], in_=xt[0:P, :], func=AF.Square,
                                                        accum_out=ss[0:P, :]), r=[xt], w=[junk, ss])
                kb.op(act, lambda: nc.scalar.activation(out=rs[0:P, :], in_=ss[0:P, :], func=AF.Sqrt, bias=epsc[0:P, :],
                                                        scale=1.0 / D), r=[ss, epsc], w=[rs])
                kb.op(dve, lambda: nc.vector.reciprocal(out=rs[0:P, :], in_=rs[0:P, :]), r=[rs], w=[rs])
                if xn_f32 is not None:
                    kb.op(dve, lambda: nc.vector.scalar_tensor_tensor(out=xn_f32[0:P, :], in0=xt[0:P, :], scalar=rs[0:P, :],
                                                                      in1=g[0:P, :], op0=ALU.mult, op1=ALU.mult),
                          r=[xt, rs, g], w=[xn_f32])
                    kb.op(pool, lambda: nc.gpsimd.tensor_copy(out=xnb[0:P, :], in_=xn_f32[0:P, :]), r=[xn_f32], w=[xnb])
                else:
                    kb.op(dve, lambda: nc.vector.scalar_tensor_tensor(out=xnb[0:P, :], in0=xt[0:P, :], scalar=rs[0:P, :],
                                                                      in1=g[0:P, :], op0=ALU.mult, op1=ALU.mult),
                          r=[xt, rs, g], w=[xnb])
                for dc in range(8):
                    kb.op(pe, (lambda dc=dc: nc.tensor.transpose(out=pT[:, dc * 128:dc * 128 + P],
                                                                 in_=xnb[0:P, dc * 128:(dc + 1) * 128],
                                                                 identity=identb[0:P, 0:P])), r=[xnb, identb], w=[pT])
                src = AP(pT[:], 0, [[D, 128], [128, 8], [1, P]])
                kb.op(act, lambda: nc.scalar.copy(out=dstT[:, :, col0:col0 + P], in_=src), r=[pT],
                      w=[dst_trk if dst_trk is not None else dstT])


        def phase0():
            kb.barrier()
            with contextlib.ExitStack() as e0:
                Wk = kb.sb([128, 8, D], BF16, es=e0, name="Wk")
                Wv = kb.sb([128, 8, D], BF16, es=e0, name="Wv")
                load_w(Wk, io["w_in"], (D, 2 * D))
                load_w(Wv, io["w_in"], (2 * D, 3 * D))
                cvt = [kb.sb([128, 16, D], BF16, es=e0, name="cvt") for _ in range(2)]
                ncv = 0
                for (src_t, off, trks) in ((io["peer_u"], 0, trk_ub), (io["peer_v"], D, trk_vb)):
                    sv = src_t.rearrange("(p j) d -> p j d", p=128)
                    dv = uv_d.rearrange("(p j) d -> p j d", p=128)[:, :, off:off + D]
                    for ch in range(8):
                        cb = cvt[ncv % 2]
                        ncv += 1
                        kb.dma(pool, (lambda cb=cb, sv=sv, ch=ch: nc.gpsimd.dma_start(out=cb[:], in_=sv[:, ch * 16:(ch + 1) * 16, :])),
                               w=[cb])
                        kb.dma(pool, (lambda cb=cb, dv=dv, ch=ch: nc.gpsimd.dma_start(out=dv[:, ch * 16:(ch + 1) * 16, :], in_=cb[:])),
                               r=[cb], w=[trks[ch]])
                nt = NormT(e0)
                xts = [kb.sb([128, D], F32, es=e0, name="xt") for _ in range(3)]
                xnT = [kb.sb([128, 8, 512], BF16, es=e0, name="xnT") for _ in range(2)]
                kTs = [kb.sb([128, 8, 512], BF16, es=e0, name="kTs") for _ in range(2)]
                vs = [kb.sb([128, 4, D], BF16, es=e0, name="vs") for _ in range(2)]
                psK = [kb.ps([128, 512], F32, es=e0, name="psK") for _ in range(2)]
                psV = [kb.ps([128, 512], F32, es=e0, name="psV") for _ in range(2)]
                kms = kb.sb([128, 8, 16], F32, es=e0, name="kms")
                xi = 0
                if STOP == 1: return
                for G in range(8):
                    xT = xnT[G % 2]
                    for tt in range(4):
                        xt = xts[xi % 3]
                        xi += 1
                        r0 = G * 512 + tt * 128
                        kb.dma(sp, (lambda xt=xt, r0=r0: nc.sync.dma_start(out=xt[:], in_=io["xfull"][r0:r0 + 128, :])), w=[xt])
                        nt.run(xt, 128, "mix_norm_g", xT, tt * 128)
                        if STOP == 2: return
                    if STOP == 3: return
                    kT = kTs[G % 2]
                    for h in range(8):
                        ps = psK[h % 2]
                        for dc in range(8):
                            kb.op(pe, (lambda dc=dc, h=h, ps=ps: nc.tensor.matmul(ps[:], lhsT=Wk[:, dc, h * 128:(h + 1) * 128],
                                                                                rhs=xT[:, dc, :], start=(dc == 0), stop=(dc == 7))),
                                  r=[Wk, xT], w=[ps])
                        for bb in range(2):
                            kb.op(act, (lambda h=h, ps=ps, bb=bb: nc.scalar.activation(
                                out=kT[:, h, bb * 256:(bb + 1) * 256], in_=ps[:, bb * 256:(bb + 1) * 256], func=AF.Copy,
                                accum_out=kms[:, h, G * 2 + bb:G * 2 + bb + 1])), r=[ps], w=[kT, kms])
                    if STOP in (4, 41): return
                    vv = vs[G % 2]
                    for tt in range(4):
                        for half in range(2):
                            ps = psV[half]
                            for dc in range(8):
                                kb.op(pe, (lambda dc=dc, tt=tt, half=half, ps=ps: nc.tensor.matmul(
                                    ps[:], lhsT=xT[:, dc, tt * 128:(tt + 1) * 128], rhs=Wv[:, dc, half * 512:(half + 1) * 512],
                                    start=(dc == 0), stop=(dc == 7))), r=[Wv, xT], w=[ps])
                            kb.op(dve, (lambda tt=tt, half=half, ps=ps: nc.vector.tensor_copy(
                                out=vv[:, tt, half * 512:(half + 1) * 512], in_=ps[:])), r=[ps], w=[vv])
                    if STOP == 5: return
                    kb.dma(sp, (lambda G=G, kT=kT: nc.sync.dma_start(
                        out=kT_d.rearrange("h p t -> p h t")[:, :, G * 512:(G + 1) * 512], in_=kT[:])), r=[kT], w=[trk_kT[G]])
                    kb.dma(sp, (lambda G=G, vv=vv: nc.sync.dma_start(
                        out=v_d.rearrange("(n p) c -> p n c", p=128)[:, G * 4:(G + 1) * 4, :], in_=vv[:])), r=[vv], w=[trk_v[G]])
                kb.op(act, lambda: nc.scalar.mul(out=kmeanT[:], in_=kms[:], mul=1.0 / 256.0), r=[kms], w=[kmeanT])

        if "p0" in PHASES:
            phase0()

        def phase1a():
            kb.barrier()
            with contextlib.ExitStack() as e1:
                Wa = kb.sb([128, 8, D], BF16, es=e1, name="Wa")
                Wb = kb.sb([128, 8, D], BF16, es=e1, name="Wb")
                Wco = kb.sb([128, 8, D], BF16, es=e1, name="Wco")
                load_w(Wa, io["w_in"], (3 * D, 4 * D))
                load_w(Wb, io["w_in"], (4 * D, 5 * D))
                load_w(Wco, io["w_conv_out"], (0, D))
                dwb = colvec(io["conv_dw_b"], e1)
                lng = colvec(io["conv_ln_g"], e1)
                lnb = colvec(io["conv_ln_b"], e1)
                bco = colvec(io["b_conv_out"], e1)
                wdw = kb.sb([128, 31, 8], F32, es=e1, name="wdw")
                for k0 in range(0, 31, 8):
                    k1 = min(31, k0 + 8)
                    kb.dma(sp, (lambda k0=k0, k1=k1: nc.sync.dma_start(
                        out=wdw[:, k0:k1, :], in_=io["conv_dw_w"][k0:k1, :].rearrange("k (c p) -> p k c", p=128),
                        allow_slow_non_contiguous=True)), w=[wdw])
                diag = kb.sb([128, 8, 31, 128], BF16, es=e1, name="diag")
                for cc in range(8):
                    for k in range(31):
                        if k % 2 == 0:
                            kb.op(dve, (lambda cc=cc, k=k: nc.vector.tensor_scalar(
                                out=diag[:, cc, k, :], in0=identf[:], scalar1=wdw[:, k, cc:cc + 1], scalar2=None, op0=ALU.mult)),
                                r=[identf, wdw], w=[diag.k((cc, k))])
                        else:
                            kb.op(act, (lambda cc=cc, k=k: nc.scalar.activation(
                                out=diag[:, cc, k, :], in_=identf[:], func=AF.Copy, scale=wdw[:, k, cc:cc + 1])),
                                r=[identf, wdw], w=[diag.k((cc, k))])
                onesm = kb.sb([128, 128], BF16, es=e1, name="onesm")
                kb.op(dve, lambda: nc.vector.memset(onesm[:], 1.0 / D), w=[onesm])
                nt = NormT(e1)
                xts = [kb.sb([128, D], F32, es=e1, name="xt") for _ in range(2)]
                xnT = [kb.sb([128, 8, 288], BF16, es=e1, name="xnT") for _ in range(2)]
                uT = [kb.sb([128, 8, 288], BF16, es=e1, name="uT") for _ in range(2)]
                sgb = [kb.sb([128, 288], F32, es=e1, name="sgb") for _ in range(2)]
                yT = kb.sb([128, 8, 256], F32, es=e1, name="yT")
                yb = kb.sb([128, 8, 256], BF16, es=e1, name="yb")
                ysq = kb.sb([128, 8, 256], BF16, es=e1, name="ysq")
                actT = kb.sb([128, 8, 256], BF16, es=e1, name="actT")
                coT = [kb.sb([128, 8, 256], BF16, es=e1, name="coT") for _ in range(1)]
                st = {n: kb.sb([128, 256], F32, es=e1, name=n) for n in ["m2", "var", "rstd", "nmr"]}
                t1 = [kb.sb([128, 256], F32, es=e1, name="t1") for _ in range(2)]
                mst = kb.sb([128, 512], F32, es=e1, name="mst")
                psA = [kb.ps([128, 512], F32, es=e1, name="psA") for _ in range(2)]
                psB = [kb.ps([128, 512], F32, es=e1, name="psB") for _ in range(2)]
                psC = [kb.ps([128, 512], F32, es=e1, name="psC") for _ in range(2)]
                xi = 0
                for i in range(8):
                    xT = xnT[i % 2]
                    for tt, P in enumerate([128, 128, 32]):
                        xt = xts[xi % 2]
                        xi += 1
                        kb.dma(sp, (lambda xt=xt, tt=tt, P=P, i=i: nc.sync.dma_start(
                            out=xt[0:P, :], in_=io["xconv"][i, tt * 128:tt * 128 + P, :])), w=[xt])
                        nt.run(xt, P, "mix_norm_g", xT, tt * 128)
                    u = uT[i % 2]
                    for cc in range(8):
                        pa, pb, sg = psA[cc % 2], psB[cc % 2], sgb[cc % 2]
                        for dc in range(8):
                            kb.op(pe, (lambda dc=dc, cc=cc, pa=pa: nc.tensor.matmul(
                                pa[:, 0:288], lhsT=Wa[:, dc, cc * 128:(cc + 1) * 128], rhs=xT[:, dc, :],
                                start=(dc == 0), stop=(dc == 7))), r=[Wa, xT], w=[pa])
                        for dc in range(8):
                            kb.op(pe, (lambda dc=dc, cc=cc, pb=pb: nc.tensor.matmul(
                                pb[:, 0:288], lhsT=Wb[:, dc, cc * 128:(cc + 1) * 128], rhs=xT[:, dc, :],
                                start=(dc == 0), stop=(dc == 7))), r=[Wb, xT], w=[pb])
                        kb.op(act, (lambda pb=pb, sg=sg: nc.scalar.activation(out=sg[:], in_=pb[:, 0:288], func=AF.Sigmoid)),
                              r=[pb], w=[sg])
                        kb.op(dve, (lambda pa=pa, sg=sg, cc=cc: nc.vector.tensor_tensor(
                            out=u[:, cc, :], in0=pa[:, 0:288], in1=sg[:], op=ALU.mult)), r=[pa, sg], w=[u])
                    for cc in range(8):
                        pc = psC[cc % 2]
                        for k in range(31):
                            kb.op(pe, (lambda cc=cc, k=k, pc=pc: nc.tensor.matmul(
                                pc[:, 0:256], lhsT=diag[:, cc, k, :], rhs=u[:, cc, 2 + k:2 + k + 256],
                                start=(k == 0), stop=(k == 30))), r=[diag.k((cc, k)), u], w=[pc])
                        kb.op(act, (lambda cc=cc, pc=pc: nc.scalar.activation(
                            out=yT[:, cc, :], in_=pc[:, 0:256], func=AF.Identity, bias=dwb[:, cc:cc + 1], scale=1.0)),
                            r=[pc, dwb], w=[yT.k(cc)])
                        kb.op(act, (lambda cc=cc, pc=pc: nc.scalar.activation(
                            out=ysq[:, cc, :], in_=pc[:, 0:256], func=AF.Square, bias=dwb[:, cc:cc + 1], scale=1.0)),
                            r=[pc, dwb], w=[ysq.k(cc)])
                        kb.op(act, (lambda cc=cc, pc=pc: nc.scalar.activation(
                            out=yb[:, cc, :], in_=pc[:, 0:256], func=AF.Identity, bias=dwb[:, cc:cc + 1], scale=1.0)),
                            r=[pc, dwb], w=[yb.k(cc)])
                    pst = psA[0]
                    for cc in range(8):
                        kb.op(pe, (lambda cc=cc: nc.tensor.matmul(pst[:, 0:256], lhsT=onesm[:], rhs=yb[:, cc, :],
                                                                 start=(cc == 0), stop=(cc == 7))), r=[onesm, yb.k(cc)], w=[pst])
                    for cc in range(8):
                        kb.op(pe, (lambda cc=cc: nc.tensor.matmul(pst[:, 256:512], lhsT=onesm[:], rhs=ysq[:, cc, :],
                                                                 start=(cc == 0), stop=(cc == 7))), r=[onesm, ysq.k(cc)], w=[pst])
                    m2, var, rstd, nmr = st["m2"], st["var"], st["rstd"], st["nmr"]
                    kb.op(act, lambda: nc.scalar.copy(out=mst[:], in_=pst[:]), r=[pst], w=[mst])
                    pst = mst
                    kb.op(dve, lambda: nc.vector.tensor_tensor(out=m2[:], in0=pst[:, 0:256], in1=pst[:, 0:256], op=ALU.mult),
                          r=[pst], w=[m2])
                    kb.op(dve, lambda: nc.vector.tensor_tensor(out=var[:], in0=pst[:, 256:512], in1=m2[:], op=ALU.subtract),
                          r=[pst, m2], w=[var])
                    kb.op(act, lambda: nc.scalar.activation(out=rstd[:], in_=var[:], func=AF.Sqrt, bias=epsc[:], scale=1.0),
                          r=[var, epsc], w=[rstd])
                    kb.op(dve, lambda: nc.vector.reciprocal(out=rstd[:], in_=rstd[:]), r=[rstd], w=[rstd])
                    kb.op(dve, lambda: nc.vector.scalar_tensor_tensor(out=nmr[:], in0=pst[:, 0:256], scalar=-1.0, in1=rstd[:],
                                                                      op0=ALU.mult, op1=ALU.mult), r=[pst, rstd], w=[nmr])
                    for cc in range(8):
                        tb = t1[cc % 2]
                        kb.op(dve, (lambda cc=cc, tb=tb: nc.vector.tensor_tensor(out=tb[:], in0=yT[:, cc, :], in1=rstd[:],
                                                                                op=ALU.mult)), r=[yT.k(cc), rstd], w=[tb])
                        kb.op(dve, (lambda cc=cc, tb=tb: nc.vector.tensor_tensor(out=tb[:], in0=tb[:], in1=nmr[:], op=ALU.add)),
                              r=[tb, nmr], w=[tb])
                        kb.op(act, (lambda cc=cc, tb=tb: nc.scalar.activation(
                            out=actT[:, cc, :], in_=tb[:], func=AF.Silu, bias=lnb[:, cc:cc + 1], scale=lng[:, cc:cc + 1])),
                            r=[tb, lnb, lng], w=[actT.k(cc)])
                    co = coT[0]
                    for nn in range(8):
                        po = psB[nn % 2]
                        for cc in range(8):
                            kb.op(pe, (lambda cc=cc, nn=nn, po=po: nc.tensor.matmul(
                                po[:, 0:256], lhsT=Wco[:, cc, nn * 128:(nn + 1) * 128], rhs=actT[:, cc, :],
                                start=(cc == 0), stop=(cc == 7))), r=[Wco, actT.k(cc)], w=[po])
                        kb.op(act, (lambda nn=nn, po=po: nc.scalar.activation(
                            out=co[:, nn, :], in_=po[:, 0:256], func=AF.Identity, bias=bco[:, nn:nn + 1], scale=1.0)),
                            r=[po, bco], w=[co])
                    kb.dma(sp, (lambda i=i, co=co: nc.sync.dma_start(
                        out=convo_d.rearrange("(nn p) t -> p nn t", p=128)[:, :, i * 256:(i + 1) * 256], in_=co[:])),
                        r=[co], w=[trk_convo[i]])


        if "p1a" in PHASES:
            phase1a()

        def phase1b():
            kb.barrier()
            with contextlib.ExitStack() as e2:
                Wq = kb.sb([128, 8, D], BF16, es=e2, name="Wq")
                Wgc = kb.sb([128, 8, D], BF16, es=e2, name="Wgc")
                Wga = kb.sb([128, 8, D], BF16, es=e2, name="Wga")
                Wao = kb.sb([128, 8, D], BF16, es=e2, name="Wao")
                Wmx = kb.sb([128, 8, D], BF16, es=e2, name="Wmx")
                load_w(Wq, io["w_in"], (0, D))
                load_w(Wgc, io["w_in"], (5 * D, 6 * D))
                load_w(Wga, io["w_in"], (6 * D, 7 * D))
                load_w(Wao, io["w_attn_out"], (0, D))
                load_w(Wmx, io["w_mix_out"], (0, D))
                krow = kb.sb([128, 8 * 512], BF16, es=e2, name="krow")
                kb.op(dve, lambda: nc.vector.memset(krow[:], 0.0), w=[krow])
                kb.dma(pool, lambda: nc.gpsimd.dma_start(out=krow[0:1, :], in_=io["krow"]), w=[krow])
                ones1 = kb.sb([128, 128], BF16, es=e2, name="ones1")
                kb.op(dve, lambda: nc.vector.memset(ones1[:], 0.0), w=[ones1])
                kb.op(dve, lambda: nc.vector.memset(ones1[0:1, :], 1.0), w=[ones1])
                cpab = kb.sb([128, 2, 512], BF16, es=e2, name="cpab")
                kb.dma(pool, lambda: nc.gpsimd.dma_start(out=cpab[:, :, 0:256], in_=io["cpa"].rearrange("s p k -> p s k")), w=[cpab])
                kb.dma(pool, lambda: nc.gpsimd.dma_start(out=cpab[:, :, 256:512], in_=io["cpb"].rearrange("s p k -> p s k")), w=[cpab])
                nt = NormT(e2, nbuf=1)
                xts = [kb.sb([128, D], F32, es=e2, name="xt") for _ in range(2)]
                xnT = kb.sb([128, 8, 256], BF16, es=e2, name="xnT")
                qT = kb.sb([128, 8, 256], BF16, es=e2, name="qT")
                sgc = kb.sb([128, 8, 256], BF16, es=e2, name="sgc")
                sga = kb.sb([128, 8, 256], BF16, es=e2, name="sga")
                coT = kb.sb([128, 8, 256], BF16, es=e2, name="coT")
                abias = kb.sb([128, 128], F32, es=e2, name="abias")
                vpen = kb.sb([128, 16], F32, es=e2, name="vpen")
                notown = kb.sb([128, 16], F32, es=e2, name="notown")
                gm = kb.sb([128, 8, 16], F32, es=e2, name="gm")
                mx8 = kb.sb([128, 8, 8], F32, es=e2, name="mx8")
                thr = kb.sb([128, 8], F32, es=e2, name="thr")
                sel = kb.sb([128, 8, 16], F32, es=e2, name="sel")
                biasall = [kb.sb([128, 8, 16], F32, es=e2, name="biasall") for _ in range(2)]
                kbuf = [kb.sb([128, 4096], BF16, es=e2, name="kbuf") for _ in range(2)]
                vbuf = [kb.sb([128, 32, 128], BF16, es=e2, name="vbuf") for _ in range(2)]
                Pb = [kb.sb([128, 4096], BF16, es=e2, name="Pb") for _ in range(1)]
                PT = [kb.sb([128, 32, 128], BF16, es=e2, name="PT") for _ in range(2)]
                zpart = [kb.sb([128, 16], F32, es=e2, name="zpart") for _ in range(2)]
                zs = [kb.sb([128, 1], F32, es=e2, name="zs") for _ in range(2)]
                zjunk = kb.sb([128, 16], F32, es=e2, name="zjunk")
                attn_tok = [kb.sb([128, D], BF16, es=e2, name="attn_tok") for _ in range(2)]
                attnT = kb.sb([128, 8, 256], BF16, es=e2, name="attnT")
                m1 = kb.sb([128, 256], F32, es=e2, name="m1")
                m2b = kb.sb([128, 256], F32, es=e2, name="m2b")
                merged = kb.sb([128, 8, 256], BF16, es=e2, name="merged")
                psP = [kb.ps([128, 512], F32, es=e2, name="psP") for _ in range(2)]
                psS = [kb.ps([128, 512], F32, es=e2, name="psS") for _ in range(2)]
                psT = [kb.ps([128, 512], BF16, es=e2, name="psT") for _ in range(2)]
                psO = kb.ps([128, 512], F32, es=e2, name="psO")
                pcnt = [0]

                def nextP():
                    pcnt[0] += 1
                    return psP[pcnt[0] % 2]

                for i in range(8):
                    NBK = 2 * i + 2
                    for s_ in range(2):
                        xt = xts[s_]
                        kb.dma(sp, (lambda xt=xt, s_=s_, i=i: nc.sync.dma_start(
                            out=xt[:], in_=io["xown"][i * 256 + s_ * 128:i * 256 + (s_ + 1) * 128, :])), w=[xt])
                        nt.run(xt, 128, "mix_norm_g", xnT, s_ * 128)
                    kb.dma(sp, (lambda i=i: nc.sync.dma_start(
                        out=coT[:], in_=convo_d.rearrange("(nn p) t -> p nn t", p=128)[:, :, i * 256:(i + 1) * 256])),
                        r=[trk_convo[i]], w=[coT])
                    for h in range(8):
                        ps = nextP()
                        for dc in range(8):
                            kb.op(pe, (lambda dc=dc, h=h, ps=ps: nc.tensor.matmul(
                                ps[:, 0:256], lhsT=Wq[:, dc, h * 128:(h + 1) * 128], rhs=xnT[:, dc, :],
                                start=(dc == 0), stop=(dc == 7))), r=[Wq, xnT], w=[ps])
                        kb.op(act, (lambda h=h, ps=ps: nc.scalar.mul(out=qT[:, h, :], in_=ps[:, 0:256], mul=128.0 ** -0.5)),
                              r=[ps], w=[qT])
                    for (Wg, sg) in ((Wgc, sgc), (Wga, sga)):
                        for nn in range(8):
                            ps = nextP()
                            for dc in range(8):
                                kb.op(pe, (lambda dc=dc, nn=nn, ps=ps, Wg=Wg: nc.tensor.matmul(
                                    ps[:, 0:256], lhsT=Wg[:, dc, nn * 128:(nn + 1) * 128], rhs=xnT[:, dc, :],
                                    start=(dc == 0), stop=(dc == 7))), r=[Wg, xnT], w=[ps])
                            kb.op(act, (lambda nn=nn, ps=ps, sg=sg: nc.scalar.activation(
                                out=sg[:, nn, :], in_=ps[:, 0:256], func=AF.Sigmoid)), r=[ps], w=[sg])
                    if STOP == 10: return
                    for s_ in range(2):
                        c = 2 * i + s_
                        qs = slice(s_ * 128, (s_ + 1) * 128)
                        kb.dma(sp, (lambda c=c: nc.sync.dma_start(out=abias[:], in_=io["abias"][c])), w=[abias])
                        kb.dma(sp, (lambda c=c: nc.sync.dma_start(out=vpen[:], in_=io["vpen"][c])), w=[vpen])
                        kb.dma(sp, (lambda c=c: nc.sync.dma_start(out=notown[:], in_=io["notown"][c])), w=[notown])
                        psg = nextP()
                        for h in range(8):
                            kb.op(pe, (lambda h=h, psg=psg, qs=qs: nc.tensor.matmul(
                                psg[:, h * 16:(h + 1) * 16], lhsT=qT[:, h, qs], rhs=kmeanT[:, h, :], start=True, stop=True)),
                                r=[qT, kmeanT], w=[psg])
                        kb.op(dve, (lambda psg=psg: nc.vector.tensor_tensor(
                            out=gm[:], in0=AP(psg[:], 0, [[512, 128], [16, 8], [1, 16]]),
                            in1=AP(vpen[:], 0, [[16, 128], [0, 8], [1, 16]]), op=ALU.add)), r=[psg, vpen], w=[gm])
                        for h in range(8):
                            kb.op(dve, (lambda h=h: nc.vector.max(out=mx8[:, h, :], in_=gm[:, h, :])), r=[gm], w=[mx8])
                        kb.op(dve, lambda: nc.vector.tensor_scalar(out=thr[:], in0=mx8[:, :, 2], scalar1=-1e29, scalar2=None,
                                                                   op0=ALU.max), r=[mx8], w=[thr])
                        kb.op(dve, lambda: nc.vector.tensor_tensor(
                            out=sel[:], in0=gm[:], in1=AP(thr[:], 0, [[8, 128], [1, 8], [0, 16]]), op=ALU.is_ge),
                            r=[gm, thr], w=[sel])
                        kb.op(dve, lambda: nc.vector.tensor_scalar(out=sel[:], in0=sel[:], scalar1=-1.0, scalar2=-NEG,
                                                                   op0=ALU.add, op1=ALU.mult), r=[sel], w=[sel])
                        kb.op(dve, lambda: nc.vector.tensor_tensor(
                            out=sel[:], in0=sel[:], in1=AP(notown[:], 0, [[16, 128], [0, 8], [1, 16]]), op=ALU.mult),
                            r=[sel, notown], w=[sel])
                        ba = biasall[s_]
                        kb.op(dve, (lambda ba=ba: nc.vector.tensor_tensor(
                            out=ba[:], in0=sel[:], in1=AP(abias[:], 0, [[128, 128], [16, 8], [1, 16]]), op=ALU.add)),
                            r=[sel, abias], w=[ba])
                    if STOP == 11: return
                    for h in range(8):
                        kbf, vbf = kbuf[h % 2], vbuf[h % 2]
                        kb.dma(sp, (lambda h=h, kbf=kbf, NBK=NBK: nc.sync.dma_start(
                            out=kbf[:, 0:NBK * 256], in_=kT_d[h, :, 0:NBK * 256])), r=trk_kT[0:(NBK * 256 + 511) // 512], w=[kbf])
                        kb.dma(sp, (lambda h=h, vbf=vbf, NBK=NBK: nc.sync.dma_start(
                            out=vbf[:, 0:NBK * 2, :],
                            in_=v_d.rearrange("(n p) c -> p n c", p=128)[:, 0:NBK * 2, h * 128:(h + 1) * 128])),
                            r=trk_v[0:(NBK * 256 + 511) // 512], w=[vbf])
                        for s_ in range(2):
                            qs = slice(s_ * 128, (s_ + 1) * 128)
                            ba = biasall[s_]
                            P_, PT_, zp, z1 = Pb[0], PT[s_], zpart[s_], zs[s_]
                            for grp in range(i + 1):
                                ps = psS[grp % 2]
                                last = (grp == i)
                                kb.op(pe, (lambda ps=ps, grp=grp, qs=qs, h=h: nc.tensor.matmul(
                                    ps[:], lhsT=qT[:, h, qs], rhs=kbf[:, grp * 512:(grp + 1) * 512], start=True, stop=False)),
                                    r=[qT, kbf], w=[ps])
                                kb.op(pe, (lambda ps=ps, h=h, last=last: nc.tensor.matmul(
                                    ps[:], lhsT=ones1[:], rhs=krow[:, h * 512:(h + 1) * 512], start=False, stop=(not last))),
                                    r=[ones1, krow], w=[ps])
                                if last:
                                    kb.op(pe, (lambda ps=ps, s_=s_: nc.tensor.matmul(
                                        ps[:], lhsT=identb[:], rhs=cpab[:, s_, :], start=False, stop=True)),
                                        r=[identb, cpab], w=[ps])
                                for bb in range(2):
                                    n = grp * 2 + bb
                                    kb.op(act, (lambda ps=ps, bb=bb, n=n, h=h, ba=ba, P_=P_, zp=zp: nc.scalar.activation(
                                        out=P_[:, n * 256:(n + 1) * 256], in_=ps[:, bb * 256:(bb + 1) * 256], func=AF.Exp,
                                        bias=ba[:, h, n:n + 1], scale=1.0, accum_out=zp[:, n:n + 1])),
                                        r=[ps, ba], w=[P_.k(n), zp.k(n)])
                            kb.op(act, (lambda zp=zp, z1=z1, NBK=NBK: nc.scalar.activation(
                                out=zjunk[:, 0:NBK], in_=zp[:, 0:NBK], func=AF.Copy, accum_out=z1[:])),
                                r=[zp.k(n_) for n_ in range(NBK)], w=[z1, zjunk])
                            kb.op(dve, (lambda z1=z1: nc.vector.reciprocal(out=z1[:], in_=z1[:])), r=[z1], w=[z1])
                            if STOP == 12:
                                dbg("biasall", ba, [128, 8, 16], F32)
                                dbg("gm", gm, [128, 8, 16], F32)
                                dbg("sel", sel, [128, 8, 16], F32)
                                dbg("mx8", mx8, [128, 8, 8], F32)
                                dbg("zp", zp, [128, 16], F32)
                                dbg("z1", z1, [128, 1], F32)
                                dbg("P", P_, [128, 4096], BF16)
                                dbg("qT", qT, [128, 8, 256], BF16)
                                dbg("kbf", kbf, [128, 4096], BF16)
                                return
                            nkt = NBK * 2
                            for k0 in range(0, nkt, 4):
                                pt = psT[(k0 // 4) % 2]
                                for kt in range(k0, k0 + 4):
                                    kb.op(pe, (lambda pt=pt, kt=kt, k0=k0, P_=P_: nc.tensor.transpose(
                                        out=pt[:, (kt - k0) * 128:(kt - k0 + 1) * 128], in_=P_[:, kt * 128:(kt + 1) * 128],
                                        identity=identb[:])), r=[P_.k(kt // 2), identb], w=[pt])
                                kb.op(dve, (lambda pt=pt, k0=k0, PT_=PT_: nc.vector.tensor_copy(
                                    out=PT_[:, k0:k0 + 4, :], in_=AP(pt[:], 0, [[512, 128], [128, 4], [1, 128]]))),
                                    r=[pt], w=[PT_.k(k0 // 4)])
                            po = psO[:, (h % 4) * 128:(h % 4 + 1) * 128]
                            for kt in range(nkt):
                                kb.op(pe, (lambda kt=kt, po=po, PT_=PT_, nkt=nkt: nc.tensor.matmul(
                                    po, lhsT=PT_[:, kt, :], rhs=vbf[:, kt, :], start=(kt == 0), stop=(kt == nkt - 1))),
                                    r=[PT_.k(kt // 4), vbf], w=[psO])
                            at = attn_tok[s_]
                            kb.op(act, (lambda po=po, at=at, h=h, z1=z1: nc.scalar.activation(
                                out=at[:, h * 128:(h + 1) * 128], in_=po, func=AF.Copy, scale=z1[:, 0:1])),
                                r=[psO, z1], w=[at])
                            if STOP == 13:
                                dbg("at", at, [128, 1024], BF16)
                                dbg("PT", PT_, [128, 32, 128], BF16)
                                dbg("vbf", vbf, [128, 32, 128], BF16)
                                dbg("z1", z1, [128, 1], F32)
                                dbg("P", P_, [128, 4096], BF16)
                                return
                    if STOP == 14: return
                    for s_ in range(2):
                        at = attn_tok[s_]
                        for k0 in range(0, 8, 4):
                            pt = psT[(k0 // 4) % 2]
                            for hc in range(k0, k0 + 4):
                                kb.op(pe, (lambda pt=pt, hc=hc, k0=k0, at=at: nc.tensor.transpose(
                                    out=pt[:, (hc - k0) * 128:(hc - k0 + 1) * 128], in_=at[:, hc * 128:(hc + 1) * 128],
                                    identity=identb[:])), r=[at, identb], w=[pt])
                            kb.op(dve, (lambda pt=pt, k0=k0, s_=s_: nc.vector.tensor_copy(
                                out=attnT[:, k0:k0 + 4, s_ * 128:(s_ + 1) * 128],
                                in_=AP(pt[:], 0, [[512, 128], [128, 4], [1, 128]]))), r=[pt], w=[attnT])
                    for nn in range(8):
                        ps = nextP()
                        for hc in range(8):
                            kb.op(pe, (lambda hc=hc, nn=nn, ps=ps: nc.tensor.matmul(
                                ps[:, 0:256], lhsT=Wao[:, hc, nn * 128:(nn + 1) * 128], rhs=attnT[:, hc, :],
                                start=(hc == 0), stop=(hc == 7))), r=[Wao, attnT], w=[ps])
                        kb.op(dve, (lambda nn=nn, ps=ps: nc.vector.tensor_tensor(out=m1[:], in0=ps[:, 0:256], in1=sga[:, nn, :],
                                                                                op=ALU.mult)), r=[ps, sga], w=[m1])
                        kb.op(dve, (lambda nn=nn: nc.vector.tensor_tensor(out=m2b[:], in0=coT[:, nn, :], in1=sgc[:, nn, :],
                                                                          op=ALU.mult)), r=[coT, sgc], w=[m2b])
                        kb.op(dve, (lambda nn=nn: nc.vector.tensor_tensor(out=merged[:, nn, :], in0=m1[:], in1=m2b[:], op=ALU.add)),
                              r=[m1, m2b], w=[merged])
                    if STOP == 15:
                        dbg("merged", merged, [128, 8, 256], BF16)
                        dbg("attnT", attnT, [128, 8, 256], BF16)
                        dbg("sga", sga, [128, 8, 256], BF16)
                        dbg("sgc", sgc, [128, 8, 256], BF16)
                        dbg("coT", coT, [128, 8, 256], BF16)
                        dbg("at0", attn_tok[0], [128, 1024], BF16)
                        dbg("at1", attn_tok[1], [128, 1024], BF16)
                        return
                    for s_ in range(2):
                        ht = xts[s_]
                        for half in range(2):
                            ps = nextP()
                            for nn in range(8):
                                kb.op(pe, (lambda nn=nn, ps=ps, s_=s_, half=half: nc.tensor.matmul(
                                    ps[:], lhsT=merged[:, nn, s_ * 128:(s_ + 1) * 128], rhs=Wmx[:, nn, half * 512:(half + 1) * 512],
                                    start=(nn == 0), stop=(nn == 7))), r=[merged, Wmx], w=[ps])
                            kb.op(dve, (lambda ps=ps, s_=s_, half=half, ht=ht: nc.vector.tensor_tensor(
                                out=ht[:, half * 512:(half + 1) * 512], in0=ps[:], in1=xts[s_][:, half * 512:(half + 1) * 512],
                                op=ALU.add)), r=[ps, xts[s_]], w=[ht])
                        kb.dma(sp, (lambda ht=ht, s_=s_, i=i: nc.sync.dma_start(
                            out=h1_d[i * 256 + s_ * 128:i * 256 + (s_ + 1) * 128, :], in_=ht[:])), r=[ht], w=[trk_h1[2 * i + s_]])

        if "p1b" in PHASES:
            phase1b()


        def phase2():
            kb.barrier()
            with contextlib.ExitStack() as e3:
                Wxq = kb.sb([128, 8, D], BF16, es=e3, name="Wxq")
                Wxo = kb.sb([128, 8, D], BF16, es=e3, name="Wxo")
                Wkv = kb.sb([128, 8, 2 * D], BF16, es=e3, name="Wkv")
                load_w(Wxq, io["w_xq"], (0, D))
                load_w(Wxo, io["w_xo"], (0, D))
                load_w(Wkv, io["w_xkv"], (0, 2 * D))
                nt = NormT(e3, nbuf=2)
                xts = [kb.sb([128, D], F32, es=e3, name="xt") for _ in range(4)]
                mnT = kb.sb([128, 8, 256], BF16, es=e3, name="mnT")
                kxT = kb.sb([128, 8, 256], BF16, es=e3, name="kxT")
                vx = kb.sb([128, 2, D], BF16, es=e3, name="vx")
                hnT = kb.sb([128, 8, 512], BF16, es=e3, name="hnT")
                qxT = kb.sb([128, 8, 512], BF16, es=e3, name="qxT")
                Px = kb.sb([128, 4, 256], BF16, es=e3, name="Px")
                PxT = kb.sb([128, 8, 128], BF16, es=e3, name="PxT")
                zx = kb.sb([128, 4], F32, es=e3, name="zx")
                o_tok = kb.sb([128, D], BF16, es=e3, name="o_tok")
                oT = kb.sb([128, 8, 128], BF16, es=e3, name="oT")
                psP = [kb.ps([128, 512], F32, es=e3, name="psP") for _ in range(2)]
                psS = [kb.ps([128, 512], F32, es=e3, name="psS") for _ in range(2)]
                psT = kb.ps([128, D], BF16, es=e3, name="psT")
                psO = kb.ps([128, 512], F32, es=e3, name="psO")
                for mt in range(2):
                    xt = xts[mt]
                    kb.dma(sp, (lambda xt=xt, mt=mt: nc.sync.dma_start(out=xt[:], in_=io["mem"][mt * 128:(mt + 1) * 128, :])), w=[xt])
                    nt.run(xt, 128, "mem_norm_g", mnT, mt * 128)
                for c in range(8):
                    ps = psP[c % 2]
                    for dc in range(8):
                        kb.op(pe, (lambda dc=dc, c=c, ps=ps: nc.tensor.matmul(
                            ps[:, 0:256], lhsT=Wkv[:, dc, c * 128:(c + 1) * 128], rhs=mnT[:, dc, :],
                            start=(dc == 0), stop=(dc == 7))), r=[Wkv, mnT], w=[ps])
                    kb.op(act, (lambda c=c, ps=ps: nc.scalar.copy(out=kxT[:, c, :], in_=ps[:, 0:256])), r=[ps], w=[kxT])
                for mt in range(2):
                    for half in range(2):
                        ps = psP[half]
                        for dc in range(8):
                            kb.op(pe, (lambda dc=dc, mt=mt, half=half, ps=ps: nc.tensor.matmul(
                                ps[:], lhsT=mnT[:, dc, mt * 128:(mt + 1) * 128], rhs=Wkv[:, dc, D + half * 512:D + (half + 1) * 512],
                                start=(dc == 0), stop=(dc == 7))), r=[Wkv, mnT], w=[ps])
                        kb.op(dve, (lambda mt=mt, half=half, ps=ps: nc.vector.tensor_copy(
                            out=vx[:, mt, half * 512:(half + 1) * 512], in_=ps[:])), r=[ps], w=[vx])
                for G in range(4):
                    for tt in range(4):
                        xt = xts[tt]
                        ti = G * 4 + tt
                        kb.dma(sp, (lambda xt=xt, ti=ti: nc.sync.dma_start(out=xt[:], in_=h1_d[ti * 128:(ti + 1) * 128, :])),
                               r=[trk_h1[ti]], w=[xt])
                        nt.run(xt, 128, "xattn_norm_g", hnT, tt * 128)
                    for c in range(8):
                        ps = psP[c % 2]
                        for dc in range(8):
                            kb.op(pe, (lambda dc=dc, c=c, ps=ps: nc.tensor.matmul(
                                ps[:], lhsT=Wxq[:, dc, c * 128:(c + 1) * 128], rhs=hnT[:, dc, :],
                                start=(dc == 0), stop=(dc == 7))), r=[Wxq, hnT], w=[ps])
                        kb.op(act, (lambda c=c, ps=ps: nc.scalar.mul(out=qxT[:, c, :], in_=ps[:], mul=1.0 / 16.0)), r=[ps], w=[qxT])
                    for tt in range(4):
                        ti = G * 4 + tt
                        xt = xts[tt]
                        ts_ = slice(tt * 128, (tt + 1) * 128)
                        for hx in range(4):
                            ps = psS[hx // 2]
                            for j in range(2):
                                kb.op(pe, (lambda ps=ps, hx=hx, j=j, ts_=ts_: nc.tensor.matmul(
                                    ps[:, (hx % 2) * 256:(hx % 2 + 1) * 256], lhsT=qxT[:, hx * 2 + j, ts_], rhs=kxT[:, hx * 2 + j, :],
                                    start=(j == 0), stop=(j == 1))), r=[qxT, kxT], w=[ps])
                            kb.op(act, (lambda ps=ps, hx=hx: nc.scalar.activation(
                                out=Px[:, hx, :], in_=ps[:, (hx % 2) * 256:(hx % 2 + 1) * 256], func=AF.Exp,
                                accum_out=zx[:, hx:hx + 1])), r=[ps], w=[Px.k(hx), zx.k(hx)])
                        for hx in range(4):
                            for mt in range(2):
                                kb.op(pe, (lambda hx=hx, mt=mt: nc.tensor.transpose(
                                    out=psT[:, (hx * 2 + mt) * 128:(hx * 2 + mt + 1) * 128], in_=Px[:, hx, mt * 128:(mt + 1) * 128],
                                    identity=identb[:])), r=[Px.k(hx), identb], w=[psT])
                        kb.op(dve, lambda: nc.vector.tensor_copy(out=PxT[:], in_=AP(psT[:], 0, [[D, 128], [128, 8], [1, 128]])),
                              r=[psT], w=[PxT])
                        kb.op(dve, lambda: nc.vector.reciprocal(out=zx[:], in_=zx[:]), r=[zx.k(hx_) for hx_ in range(4)],
                              w=[zx.k(hx_) for hx_ in range(4)])
                        for hh in range(2):
                            for hx in (2 * hh, 2 * hh + 1):
                                for mt in range(2):
                                    kb.op(pe, (lambda hx=hx, mt=mt: nc.tensor.matmul(
                                        psO[:, (hx % 2) * 256:(hx % 2 + 1) * 256], lhsT=PxT[:, hx * 2 + mt, :],
                                        rhs=vx[:, mt, hx * 256:(hx + 1) * 256], start=(mt == 0), stop=(mt == 1))),
                                        r=[PxT, vx], w=[psO])
                                kb.op(act, (lambda hx=hx: nc.scalar.activation(
                                    out=o_tok[:, hx * 256:(hx + 1) * 256], in_=psO[:, (hx % 2) * 256:(hx % 2 + 1) * 256],
                                    func=AF.Copy, scale=zx[:, hx:hx + 1])), r=[psO, zx.k(hx)], w=[o_tok])
                        for c in range(8):
                            kb.op(pe, (lambda c=c: nc.tensor.transpose(out=psT[:, c * 128:(c + 1) * 128],
                                                                       in_=o_tok[:, c * 128:(c + 1) * 128], identity=identb[:])),
                                  r=[o_tok, identb], w=[psT])
                        kb.op(dve, lambda: nc.vector.tensor_copy(out=oT[:], in_=AP(psT[:], 0, [[D, 128], [128, 8], [1, 128]])),
                              r=[psT], w=[oT])
                        for half in range(2):
                            ps = psP[half]
                            for c in range(8):
                                kb.op(pe, (lambda c=c, half=half, ps=ps: nc.tensor.matmul(
                                    ps[:], lhsT=oT[:, c, :], rhs=Wxo[:, c, half * 512:(half + 1) * 512],
                                    start=(c == 0), stop=(c == 7))), r=[oT, Wxo], w=[ps])
                            kb.op(dve, (lambda half=half, ps=ps, xt=xt: nc.vector.tensor_tensor(
                                out=xt[:, half * 512:(half + 1) * 512], in0=ps[:], in1=xt[:, half * 512:(half + 1) * 512],
                                op=ALU.add)), r=[ps, xt], w=[xt])
                        kb.dma(sp, (lambda xt=xt, ti=ti: nc.sync.dma_start(out=h2_d[ti * 128:(ti + 1) * 128, :], in_=xt[:])),
                               r=[xt], w=[trk_h2[ti]])

        if "p2" in PHASES:
            phase2()


        def phase3():
            kb.barrier()
            with contextlib.ExitStack() as e4:
                Wpq = kb.sb([128, 8, 2 * D], BF16, es=e4, name="Wpq")
                load_w(Wpq, io["w_peer_q"], (0, 2 * D))
                nt = NormT(e4, nbuf=2, npsum=1)
                gfin = nt.getg("final_norm_g")
                keysT = kb.sb([128, 16, 128], BF16, es=e4, name="keysT")
                ktmp = [kb.sb([128, 128], BF16, es=e4, name="ktmp") for _ in range(2)]
                xts = [kb.sb([128, D], F32, es=e4, name="xt") for _ in range(2)]
                xpTs = [kb.sb([128, 8, 128], BF16, es=e4, name="xpT") for _ in range(2)]
                qpT = kb.sb([128, 16, 128], BF16, es=e4, name="qpT")
                s_sb = kb.sb([128, 16, 128], F32, es=e4, name="s_sb")
                s2_sb = kb.sb([128, 16, 128], F32, es=e4, name="s2_sb")
                v16 = kb.sb([128, 16, 16], F32, es=e4, name="v16")
                ix16 = kb.sb([128, 16, 16], U32, es=e4, name="ix16")
                cand = kb.sb([128, 8, 256], F32, es=e4, name="cand")
                cand2 = kb.sb([128, 8, 256], F32, es=e4, name="cand2")
                sc = kb.sb([128, 8, 16], F32, es=e4, name="sc")
                ci = kb.sb([128, 8, 16], U32, es=e4, name="ci")
                abu = kb.sb([128, 2, 128], U32, es=e4, name="abu")
                abf = kb.sb([128, 2, 128], F32, es=e4, name="abf")
                i12f = kb.sb([128, 2, 128], F32, es=e4, name="i12f")
                eq = kb.sb([128, 8, 16, 16], F32, es=e4, name="eq")
                i12b = kb.sb([128, 2, 128], BF16, es=e4, name="i12b")
                i12T = kb.sb([128, 2, 128], F32, es=e4, name="i12T")
                eTf = kb.sb([128, 128], F32, es=e4, name="eTf")
                eTi = [kb.sb([128, 128], I32, es=e4, name="eTi") for _ in range(2)]
                dsc = kb.sb([128, 8, 16], F32, es=e4, name="dsc")
                ex = kb.sb([128, 8, 16], F32, es=e4, name="ex")
                zsum = kb.sb([128, 8], F32, es=e4, name="zsum")
                gw = kb.sb([128, 8, 16], F32, es=e4, name="gw")
                gwT = [kb.sb([128, 128], F32, es=e4, name="gwT") for _ in range(2)]
                iota = kb.sb([128, 16], F32, es=e4, name="iota")
                kb.dma(sp, lambda: nc.sync.dma_start(out=iota[:], in_=io["iota16"]), w=[iota])
                uvr = [kb.sb([128, 2 * D], BF16, es=e4, name="uvr") for _ in range(16)]
                zg = [kb.sb([128, 8, 128], BF16, es=e4, name="zg") for _ in range(2)]
                wband = kb.sb([128, 8, 248], BF16, es=e4, name="wband")
                kb.op(dve, lambda: nc.vector.memset(wband[:], 0.0), w=[wband])
                for j_ in range(8):
                    kb.op(dve, (lambda j_=j_: nc.vector.memset(wband[:, j_, 120 + j_:121 + j_], 1.0)), w=[wband])
                junk5 = kb.sb([128, D], BF16, es=e4, name="junk5")
                adot = kb.sb([128, 128, 2], F32, es=e4, name="adot")
                asum = kb.sb([128, 8], F32, es=e4, name="asum")
                agl = kb.sb([128, 8], F32, es=e4, name="agl")
                cT = [kb.sb([128, 8], BF16, es=e4, name="cT") for _ in range(2)]
                ssf = kb.sb([128, 1], F32, es=e4, name="ssf")
                rsf = kb.sb([128, 1], F32, es=e4, name="rsf")
                psA1 = kb.ps([128, 512], F32, es=e4, name="psA")
                psA = [psA1, psA1]
                psTb = nt.pT[0]
                psU = [kb.ps([128, D], BF16, es=e4, name="psU") for _ in range(2)]
                psAd = kb.ps([128, 512], F32, es=e4, name="psAd")
                UTs = [kb.sb([128, 8, 128], BF16, es=e4, name="UTs") for _ in range(3)]
                psY = kb.ps([128, D], F32, es=e4, name="psY")
                for c in range(16):
                    kt_ = ktmp[c % 2]
                    kb.dma(pool, (lambda c=c, kt_=kt_: nc.gpsimd.dma_start(out=kt_[:], in_=io["peer_subkeys"][c])), w=[kt_])
                    kb.op(pe, (lambda c=c, kt_=kt_: nc.tensor.transpose(out=psTb[:, (c % 4) * 128:(c % 4 + 1) * 128], in_=kt_[:],
                                                                        identity=identb[:])), r=[kt_, identb], w=[psTb])
                    if c % 4 == 3:
                        kb.op(dve, (lambda c=c: nc.vector.tensor_copy(
                            out=keysT[:, c - 3:c + 1, :], in_=AP(psTb[:], 0, [[D, 128], [128, 4], [1, 128]]))), r=[psTb], w=[keysT])
                gi_box = [0]

                def prologue(ti):
                    xt = xts[ti % 2]
                    kb.dma(sp, (lambda xt=xt, ti=ti: nc.sync.dma_start(out=xt[:], in_=h2_d[ti * 128:(ti + 1) * 128, :])),
                           r=[trk_h2[ti]], w=[xt])
                    xpT = xpTs[ti % 2]
                    nt.run(xt, 128, "ffn_norm_g", xpT, 0)
                    xnb = xpT
                    for c in range(16):
                        ps = psA[(c // 4) % 2]
                        for dc in range(8):
                            kb.op(pe, (lambda dc=dc, c=c, ps=ps: nc.tensor.matmul(
                                ps[:, (c % 4) * 128:(c % 4 + 1) * 128], lhsT=Wpq[:, dc, c * 128:(c + 1) * 128], rhs=xpT[:, dc, :],
                                start=(dc == 0), stop=(dc == 7))), r=[Wpq, xpT], w=[ps])
                        if c % 4 == 3:
                            kb.op(act, (lambda c=c, ps=ps: nc.scalar.copy(
                                out=qpT[:, c - 3:c + 1, :], in_=AP(ps[:], 0, [[512, 128], [128, 4], [1, 128]]))), r=[ps], w=[qpT.k(c // 4)])
                    for cg in range(4):
                        ps = psA[cg % 2]
                        for c in range(cg * 4, cg * 4 + 4):
                            kb.op(pe, (lambda c=c, ps=ps: nc.tensor.matmul(
                                ps[:, (c % 4) * 128:(c % 4 + 1) * 128], lhsT=qpT[:, c, :], rhs=keysT[:, c, :], start=True, stop=True)),
                                r=[qpT.k(c // 4), keysT], w=[ps])
                        kb.op(act, (lambda cg=cg, ps=ps: nc.scalar.copy(
                            out=s_sb[:, cg * 4:cg * 4 + 4, :], in_=AP(ps[:], 0, [[512, 128], [128, 4], [1, 128]]))), r=[ps], w=[s_sb.k(cg)])
                    for c in range(16):
                        kb.op(dve, (lambda c=c: nc.vector.max(out=v16[:, c, 0:8], in_=s_sb[:, c, :])), r=[s_sb.k(c // 4)], w=[v16])
                        kb.op(dve, (lambda c=c: nc.vector.max_index(out=ix16[:, c, 0:8], in_max=v16[:, c, 0:8], in_values=s_sb[:, c, :])),
                              r=[s_sb.k(c // 4), v16], w=[ix16])
                        kb.op(dve, (lambda c=c: nc.vector.match_replace(out=s2_sb[:, c, :], in_to_replace=v16[:, c, 0:8],
                                                                        in_values=s_sb[:, c, :], imm_value=-1e30)),
                              r=[s_sb.k(c // 4), v16], w=[s2_sb])
                        kb.op(dve, (lambda c=c: nc.vector.max(out=v16[:, c, 8:16], in_=s2_sb[:, c, :])), r=[s2_sb], w=[v16])
                        kb.op(dve, (lambda c=c: nc.vector.max_index(out=ix16[:, c, 8:16], in_max=v16[:, c, 8:16], in_values=s2_sb[:, c, :])),
                              r=[s2_sb, v16], w=[ix16])
                    for h in range(8):
                        kb.op(dve, (lambda h=h: nc.vector.tensor_tensor(
                            out=AP(cand[:], h * 256, [[2048, 128], [16, 16], [1, 16]]),
                            in0=AP(v16[:], h * 32, [[256, 128], [1, 16], [0, 16]]),
                            in1=AP(v16[:], h * 32 + 16, [[256, 128], [0, 16], [1, 16]]), op=ALU.add)), r=[v16], w=[cand])
                    for h in range(8):
                        kb.op(dve, (lambda h=h: nc.vector.max(out=sc[:, h, 0:8], in_=cand[:, h, :])), r=[cand], w=[sc])
                        kb.op(dve, (lambda h=h: nc.vector.max_index(out=ci[:, h, 0:8], in_max=sc[:, h, 0:8], in_values=cand[:, h, :])),
                              r=[cand, sc], w=[ci])
                        kb.op(dve, (lambda h=h: nc.vector.match_replace(out=cand2[:, h, :], in_to_replace=sc[:, h, 0:8],
                                                                        in_values=cand[:, h, :], imm_value=-1e30)),
                              r=[cand, sc], w=[cand2])
                        kb.op(dve, (lambda h=h: nc.vector.max(out=sc[:, h, 8:16], in_=cand2[:, h, :])), r=[cand2], w=[sc])
                        kb.op(dve, (lambda h=h: nc.vector.max_index(out=ci[:, h, 8:16], in_max=sc[:, h, 8:16], in_values=cand2[:, h, :])),
                              r=[cand2, sc], w=[ci])
                    civ = AP(ci[:], 0, [[128, 128], [1, 128]])
                    kb.op(dve, lambda: nc.vector.tensor_single_scalar(out=abu[:, 0, :], in_=civ, scalar=4, op=ALU.logical_shift_right),
                          r=[ci], w=[abu])
                    kb.op(dve, lambda: nc.vector.tensor_single_scalar(out=abu[:, 1, :], in_=civ, scalar=15, op=ALU.bitwise_and),
                          r=[ci], w=[abu])
                    kb.op(dve, lambda: nc.vector.tensor_copy(out=abf[:], in_=abu[:]), r=[abu], w=[abf])
                    for hf in range(2):
                        kb.op(dve, (lambda hf=hf: nc.vector.tensor_copy(
                            out=AP(i12f[:], hf * 128, [[256, 128], [16, 8], [1, 16]]),
                            in_=AP(ix16[:], hf * 16, [[256, 128], [32, 8], [1, 16]]))), r=[ix16], w=[i12f])
                    for hf in range(2):
                        for h in range(8):
                            kb.op(dve, (lambda hf=hf, h=h: nc.vector.tensor_tensor(
                                out=eq[:, h, :, :], in0=AP(abf[:], hf * 128 + h * 16, [[256, 128], [1, 16], [0, 16]]),
                                in1=AP(iota[:], 0, [[16, 128], [0, 16], [1, 16]]), op=ALU.is_equal)), r=[abf, iota], w=[eq])
                            kb.op(dve, (lambda hf=hf, h=h: nc.vector.tensor_tensor(
                                out=eq[:, h, :, :], in0=eq[:, h, :, :],
                                in1=AP(i12f[:], hf * 128 + h * 16, [[256, 128], [0, 16], [1, 16]]), op=ALU.mult)), r=[eq, i12f], w=[eq])
                        for wd in (8, 4, 2, 1):
                            kb.op(dve, (lambda wd=wd: nc.vector.tensor_tensor(
                                out=AP(eq[:], 0, [[2048, 128], [16, 128], [1, wd]]), in0=AP(eq[:], 0, [[2048, 128], [16, 128], [1, wd]]),
                                in1=AP(eq[:], wd, [[2048, 128], [16, 128], [1, wd]]), op=ALU.add)), r=[eq], w=[eq])
                        kb.op(dve, (lambda hf=hf: nc.vector.tensor_copy(
                            out=i12b[:, hf, :], in_=AP(eq[:], 0, [[2048, 128], [16, 128]]))), r=[eq], w=[i12b])
                    for hf in range(2):
                        kb.op(pe, (lambda hf=hf: nc.tensor.transpose(out=psTb[:, hf * 128:(hf + 1) * 128], in_=i12b[:, hf, :],
                                                                     identity=identb[:])), r=[i12b, identb], w=[psTb])
                    kb.op(act, lambda: nc.scalar.copy(out=AP(i12T[:], 0, [[256, 128], [1, 256]]), in_=psTb[:, 0:256]), r=[psTb], w=[i12T])
                    kb.op(dve, lambda: nc.vector.scalar_tensor_tensor(out=eTf[:], in0=i12T[:, 0, :], scalar=128.0, in1=i12T[:, 1, :],
                                                                      op0=ALU.mult, op1=ALU.add), r=[i12T], w=[eTf])
                    eT = eTi[ti % 2]
                    kb.op(dve, (lambda eT=eT: nc.vector.tensor_copy(out=eT[:], in_=eTf[:])), r=[eTf], w=[eT])
                    kb.op(dve, lambda: nc.vector.tensor_tensor(out=dsc[:], in0=sc[:], in1=AP(sc[:], 0, [[128, 128], [16, 8], [0, 16]]),
                                                               op=ALU.subtract), r=[sc], w=[dsc])
                    for h in range(8):
                        kb.op(act, (lambda h=h: nc.scalar.activation(out=ex[:, h, :], in_=dsc[:, h, :], func=AF.Exp,
                                                                     accum_out=zsum[:, h:h + 1])), r=[dsc], w=[ex, zsum])
                    kb.op(dve, lambda: nc.vector.reciprocal(out=zsum[:], in_=zsum[:]), r=[zsum], w=[zsum])
                    kb.op(dve, lambda: nc.vector.tensor_tensor(out=gw[:], in0=ex[:], in1=AP(zsum[:], 0, [[8, 128], [1, 8], [0, 16]]),
                                                               op=ALU.mult), r=[ex, zsum], w=[gw])
                    psg = psA[0]
                    kb.op(pe, lambda: nc.tensor.transpose(out=psg[:, 0:128], in_=AP(gw[:], 0, [[128, 128], [1, 128]]), identity=identf[:]),
                          r=[gw, identf], w=[psg])
                    gT = gwT[ti % 2]
                    kb.op(act, (lambda gT=gT: nc.scalar.copy(out=gT[:], in_=psg[:, 0:128])), r=[psg], w=[gT])
                    return xt, xnb, eT, gT

                def tokens(ti, xt, xnb, eT, gT, th):
                    gi = gi_box[0]
                    def finalize(g8, toks, gT=gT):
                        cTg = cT[g8 % 2]
                        ZG = zg[g8 % 2]
                        kb.op(act, lambda: nc.scalar.activation(out=agl[:], in_=psAd[:, g8 * 8:(g8 + 1) * 8], func=AF.Gelu),
                              r=[psAd.k(g8)], w=[agl])
                        kb.op(dve, (lambda: nc.vector.tensor_tensor(
                            out=cTg[:], in0=agl[:], in1=gT[:, g8 * 8:(g8 + 1) * 8], op=ALU.mult)), r=[agl, gT], w=[cTg])
                        kb.op(dve, (lambda: nc.vector.tensor_tensor(
                            out=ZG[:], in0=wband[:, :, 120 - 8 * g8:248 - 8 * g8], in1=AP(cTg[:], 0, [[8, 128], [1, 8], [0, 128]]),
                            op=ALU.mult)), r=[wband, cTg], w=[ZG])
                        return ZG

                    def vmm(g8, toks, ZG, j):
                        t = g8 * 8 + j
                        v_ = toks[j]
                        for half in range(2):
                            kb.op(pe, (lambda v_=v_, half=half, t=t, j=j: nc.tensor.matmul(
                                psY[:, half * 512:(half + 1) * 512], lhsT=ZG[:, j, :], rhs=v_[:, D + half * 512:D + (half + 1) * 512],
                                start=(t == 0), stop=(t == 127))), r=[ZG, v_], w=[psY])

                    xpT = xnb

                    def dotmm(pm):
                        t_, us_, g_ = pm
                        for dc in range(8):
                            kb.op(pe, (lambda dc=dc, t_=t_, us_=us_: nc.tensor.matmul(
                                psAd[:, t_:t_ + 1], lhsT=us_[:, dc, :], rhs=xpT[:, dc, t_:t_ + 1], start=(dc == 0), stop=(dc == 7))),
                                r=[us_, xpT], w=[psAd.k(g_)])

                    prev = None
                    pend = None
                    pZG = None
                    for g8 in range(16):
                        toks = []
                        for j in range(8):
                            t = g8 * 8 + j
                            uv_ = uvr[gi % len(uvr)]
                            toks.append(uv_)
                            gi += 1
                            kb.dma(pool, (lambda uv_=uv_, t=t, eT=eT: nc.gpsimd.indirect_dma_start(
                                out=uv_[:], out_offset=None, in_=uv_d,
                                in_offset=bass.IndirectOffsetOnAxis(ap=eT[:, t:t + 1], axis=0))), r=[eT] + trk_ub + trk_vb, w=[uv_])
                            if j == 0 and pend is not None:
                                dotmm(pend)
                                pend = None
                                pZG = finalize(prev[0], prev[1])
                            pu = psU[t % 2]
                            for dc in range(8):
                                kb.op(pe, (lambda dc=dc, pu=pu, uv_=uv_: nc.tensor.transpose(
                                    out=pu[:, dc * 128:(dc + 1) * 128], in_=uv_[:, dc * 128:(dc + 1) * 128], identity=identb[:])),
                                    r=[uv_, identb], w=[pu])
                            us = UTs[t % 3]
                            kb.op(act, (lambda pu=pu, us=us: nc.scalar.copy(out=AP(us[:], 0, [[D, 128], [1, D]]), in_=pu[:])), r=[pu], w=[us])
                            if pend is not None:
                                dotmm(pend)
                            pend = (t, us, g8)
                            if prev is not None and j >= 2:
                                vmm(prev[0], prev[1], pZG, j - 2)
                            kb.flush(th, 5)
                        if prev is not None:
                            vmm(prev[0], prev[1], pZG, 6)
                            vmm(prev[0], prev[1], pZG, 7)
                        prev = (g8, toks)
                    dotmm(pend)
                    pZG = finalize(prev[0], prev[1])
                    for j in range(8):
                        vmm(prev[0], prev[1], pZG, j)
                    for half in range(2):
                        kb.op(dve, (lambda half=half, xt=xt: nc.vector.tensor_tensor(
                            out=xt[:, half * 512:(half + 1) * 512], in0=psY[:, half * 512:(half + 1) * 512],
                            in1=xt[:, half * 512:(half + 1) * 512], op=ALU.add)), r=[psY, xt], w=[xt])
                    kb.op(act, (lambda xt=xt: nc.scalar.activation(out=nt.junk[0][:], in_=xt[:], func=AF.Square, accum_out=ssf[:])),
                          r=[xt], w=[nt.junk[0], ssf])
                    kb.op(act, lambda: nc.scalar.activation(out=rsf[:], in_=ssf[:], func=AF.Sqrt, bias=epsc[:], scale=1.0 / D),
                          r=[ssf, epsc], w=[rsf])
                    kb.op(dve, lambda: nc.vector.reciprocal(out=rsf[:], in_=rsf[:]), r=[rsf], w=[rsf])
                    kb.op(dve, (lambda xt=xt: nc.vector.scalar_tensor_tensor(out=AP(eq[:], 0, [[2048, 128], [1, D]]), in0=xt[:], scalar=rsf[:], in1=gfin[:],
                                                                             op0=ALU.mult, op1=ALU.mult)), r=[xt, rsf, gfin], w=[eq])
                    kb.dma(sp, (lambda ti=ti: nc.sync.dma_start(out=out[ti * 128:(ti + 1) * 128, :], in_=AP(eq[:], 0, [[2048, 128], [1, D]]))), r=[eq], w=[trk_out])
                    gi_box[0] = gi

                kb.defer = []
                st = prologue(0)
                th = kb.defer
                kb.defer = None
                kb.flush(th)
                for ti in range(16):
                    th = []
                    if ti < 15:
                        kb.defer = []
                        st_next = prologue(ti + 1)
                        th = kb.defer
                        kb.defer = None
                    tokens(ti, st[0], st[1], st[2], st[3], th)
                    kb.flush(th)
                    if ti < 15:
                        st = st_next

        if "p3" in PHASES:
            phase3()

        PHASE_REST(locals())

        kb.finish(trk_kT + trk_v + trk_convo + trk_h1 + trk_h2 + [trk_out] + dbg_trks + trk_ub + trk_vb)
    return nc


PHASES = ("p0", "p1a", "p1b", "p2", "p3")
STOP = 0
NO_SELF_WAIT = ("pe",)
DEBUG = False


class StopBuild(Exception):
    pass


def ck(n):
    if STOP == n:
        raise StopBuild()

DBG_KIND = {}


def PHASE_REST(L):
    pass


def _const_tables(par):
    slopes = 2.0 ** (-np.arange(1, 9, dtype=np.float64))
    abias = np.zeros((16, 128, 8, 16), np.float32)
    vpen = np.zeros((16, 128, 16), np.float32)
    notown = np.ones((16, 128, 16), np.float32)
    p = np.arange(128)
    for c in range(16):
        i, s = c // 2, c % 2
        g = 2 * i + par
        t = g * 256 + s * 128 + p
        for n in range(16):
            if n > g:
                abias[c, :, :, n] = NEG
                vpen[c, :, n] = -1e30
            else:
                abias[c, :, :, n] = -(slopes[None, :] * (t[:, None] - n * 256))
                if n == g:
                    vpen[c, :, n] = -1e30
                    notown[c, :, n] = 0.0
    kl = np.arange(256)
    causal = np.zeros((2, 128, 256), np.float32)
    for s in range(2):
        causal[s] = np.where(kl[None, :] <= (s * 128 + p)[:, None], 0.0, NEG)
    cpa = causal if par == 0 else np.zeros_like(causal)
    cpb = causal
    krow = np.zeros((8, 512), np.float32)
    for h in range(8):
        krow[h] = slopes[h] * (np.arange(512) % 256)
    return dict(abias=abias.reshape(16, 128, 128), vpen=vpen, notown=notown, cpa=cpa, cpb=cpb,
                krow=krow.reshape(1, 8 * 512), identf=np.eye(128, dtype=np.float32),
                iota16=np.tile(np.arange(16, dtype=np.float32), (128, 1)))


def make_in_maps(inputs, cores=range(8)):
    x = np.asarray(inputs["x"], np.float32)
    mem = np.asarray(inputs["mem"], np.float32)
    shared = {}
    for nm in ["mix_norm_g", "w_in", "conv_dw_w", "conv_dw_b", "conv_ln_g", "conv_ln_b", "w_conv_out", "b_conv_out",
               "w_attn_out", "w_mix_out", "xattn_norm_g", "mem_norm_g", "w_xq", "w_xkv", "w_xo", "ffn_norm_g",
               "w_peer_q", "peer_u", "peer_v"]:
        shared[nm] = np.ascontiguousarray(np.asarray(inputs[nm], np.float32)[0])
    shared["peer_subkeys"] = np.ascontiguousarray(np.asarray(inputs["peer_subkeys"], np.float32)[0].reshape(16, 128, 128))
    shared["final_norm_g"] = np.ascontiguousarray(np.asarray(inputs["final_norm_g"], np.float32))
    maps = []
    for c in cores:
        b, par = c // 2, c % 2
        xb = x[b]
        blocks = [2 * i + par for i in range(8)]
        xown = np.concatenate([xb[g * 256:(g + 1) * 256] for g in blocks], axis=0)
        xconv = np.zeros((8, 288, D), np.float32)
        for i, g in enumerate(blocks):
            lo = g * 256 - 32
            if lo >= 0:
                xconv[i] = xb[lo:lo + 288]
            else:
                xconv[i, 32:] = xb[0:256]
        m = dict(shared)
        m.update(_const_tables(par))
        m["xfull"] = np.ascontiguousarray(xb)
        m["xown"] = np.ascontiguousarray(xown)
        m["xconv"] = xconv
        m["mem"] = np.ascontiguousarray(mem[b])
        maps.append(m)
    return maps


def kernel(**inputs):
    nc = build_program()
    maps = make_in_maps(inputs)
    res = run_bass_kernel_spmd(nc, maps, core_ids=list(range(8)))
    out = np.zeros((4, SEQ, D), np.float32)
    for c in range(8):
        b, par = c // 2, c % 2
        o = np.asarray(res.results[c]["out"], np.float32)
        for i in range(8):
            g = 2 * i + par
            out[b, g * 256:(g + 1) * 256] = o[i * 256:(i + 1) * 256]
    return out
```

```python
import contextlib
import numpy as np
import ml_dtypes
import concourse.bass as bass
import concourse.mybir as mybir
from concourse.bass_utils import run_bass_kernel_spmd

F32 = mybir.dt.float32
BF16 = mybir.dt.bfloat16
U32 = mybir.dt.uint32
I32 = mybir.dt.int32
ALU = mybir.AluOpType
AF = mybir.ActivationFunctionType
AX = mybir.AxisListType

D = 1024
SEQ = 4096
NB_TOK = 2048
EPS = 1e-6
NEG = -30000.0


class Trk:
    __slots__ = ("w", "r")

    def __init__(self):
        self.w = {}
        self.r = {}


class T:
    def __init__(self, t):
        self.t = t
        self.trk = Trk()
        self.sub = {}

    def __getitem__(self, k):
        return self.t[k]

    def k(self, key):
        if key not in self.sub:
            self.sub[key] = Trk()
        return self.sub[key]


class Eng:
    def __init__(self, name, eng, sem):
        self.name = name
        self.eng = eng
        self.sem = sem
        self.count = 0
        self.seen = {}


def _trk(x):
    return x.trk if isinstance(x, T) else x


class KB:
    def __init__(self, nc, es, nds=48):
        self.nc = nc
        self.es = es
        self.pe = Eng("pe", nc.tensor, es.enter_context(nc.semaphore("s_pe")))
        self.act = Eng("act", nc.scalar, es.enter_context(nc.semaphore("s_act")))
        self.dve = Eng("dve", nc.vector, es.enter_context(nc.semaphore("s_dve")))
        self.pool = Eng("pool", nc.gpsimd, es.enter_context(nc.semaphore("s_pool")))
        self.sp = Eng("sp", nc.sync, es.enter_context(nc.semaphore("s_sp")))
        self.dsems = [es.enter_context(nc.semaphore(f"s_d{i}")) for i in range(nds)]
        self.dvals = [0] * nds
        self.dnext = {"hw": 0, "sw": nds // 2}
        self.uid = 0
        self.defer = None

    def name(self, p):
        self.uid += 1
        return f"{p}_{self.uid}"

    def sb(self, shape, dt, es=None, name="sb"):
        return T((es or self.es).enter_context(self.nc.sbuf_tensor(self.name(name), list(shape), dt)))

    def ps(self, shape, dt, es=None, name="ps"):
        return T((es or self.es).enter_context(self.nc.psum_tensor(self.name(name), list(shape), dt)))

    def _deps(self, E, r, w):
        deps = {}
        for x in r:
            for sid, (sem, val) in _trk(x).w.items():
                if deps.get(sid, (None, 0))[1] < val:
                    deps[sid] = (sem, val)
        for x in w:
            tk = _trk(x)
            for dct in (tk.w, tk.r):
                for sid, (sem, val) in dct.items():
                    if deps.get(sid, (None, 0))[1] < val:
                        deps[sid] = (sem, val)
        for sid, (sem, val) in deps.items():
            if E.seen.get(sid, 0) < val:
                if sid == id(E.sem) and E.name in NO_SELF_WAIT:
                    continue
                E.eng.wait_ge(sem, val)
                E.seen[sid] = val

    def _post(self, ev, r, w):
        sid = id(ev[0])
        for x in r:
            tk = _trk(x)
            if tk.r.get(sid, (None, 0))[1] < ev[1]:
                tk.r[sid] = ev
        for x in w:
            tk = _trk(x)
            tk.w = {sid: ev}
            tk.r = {}

    def flush(self, th, n=None):
        k = 0
        while th and (n is None or k < n):
            kind, E, fn, r, w = th.pop(0)
            (self.op if kind == "op" else self.dma)(E, fn, r, w)
            k += 1

    def op(self, E, fn, r=(), w=()):
        if self.defer is not None:
            self.defer.append(("op", E, fn, list(r), list(w)))
            return None
        self._deps(E, r, w)
        ins = fn()
        E.count += 1
        ins.then_inc(E.sem, 1)
        self._post((E.sem, E.count), r, w)
        return ins

    def dma(self, Q, fn, r=(), w=()):
        if self.defer is not None:
            self.defer.append(("dma", Q, fn, list(r), list(w)))
            return None
        self._deps(Q, r, w)
        half = len(self.dsems) // 2
        kq = "sw" if Q.name == "pool" else "hw"
        j = self.dnext[kq]
        base = half if kq == "sw" else 0
        self.dnext[kq] = base + (j - base + 1) % half
        sem = self.dsems[j]
        if self.dvals[j] > 0 and Q.seen.get(id(sem), 0) < self.dvals[j]:
            Q.eng.wait_ge(sem, self.dvals[j])
            Q.seen[id(sem)] = self.dvals[j]
        ins = fn()
        self.dvals[j] += 16
        ins.then_inc(sem, 16)
        self._post((sem, self.dvals[j]), r, w)
        return ins

    def barrier(self):
        engs = (self.pe, self.act, self.dve, self.pool, self.sp)
        for E in engs:
            for F in engs:
                if F is not E and F.count > 0 and E.seen.get(id(F.sem), 0) < F.count:
                    E.eng.wait_ge(F.sem, F.count)
                    E.seen[id(F.sem)] = F.count
            for j, sem in enumerate(self.dsems):
                if self.dvals[j] > 0 and E.seen.get(id(sem), 0) < self.dvals[j]:
                    E.eng.wait_ge(sem, self.dvals[j])
                    E.seen[id(sem)] = self.dvals[j]

    def finish(self, trks):
        self._deps(self.sp, trks, ())
        for j, sem in enumerate(self.dsems):
            if self.dvals[j] > 0 and self.sp.seen.get(id(sem), 0) < self.dvals[j]:
                self.sp.eng.wait_ge(sem, self.dvals[j])
        for E in (self.pe, self.act, self.dve, self.pool):
            if E.count > 0:
                self.sp.eng.wait_ge(E.sem, E.count)


def AP(base, off, dims):
    return bass.AP(tensor=base.tensor, offset=off, ap=[list(d) for d in dims])


def build_program():
    nc = bass.Bass("TRN2", target_bir_lowering=False)

    def din(name, shape, dt=F32):
        return nc.dram_tensor(name, list(shape), dt, kind="ExternalInput").ap()

    io = {}
    io["xfull"] = din("xfull", [SEQ, D])
    io["xown"] = din("xown", [NB_TOK, D])
    io["xconv"] = din("xconv", [8, 288, D])
    io["mem"] = din("mem", [256, D])
    for nm, shp in [("mix_norm_g", [D]), ("w_in", [D, 7 * D]), ("conv_dw_w", [31, D]), ("conv_dw_b", [D]),
                    ("conv_ln_g", [D]), ("conv_ln_b", [D]), ("w_conv_out", [D, D]), ("b_conv_out", [D]),
                    ("w_attn_out", [D, D]), ("w_mix_out", [D, D]), ("xattn_norm_g", [D]), ("mem_norm_g", [D]),
                    ("w_xq", [D, D]), ("w_xkv", [D, 2 * D]), ("w_xo", [D, D]), ("ffn_norm_g", [D]),
                    ("w_peer_q", [D, 2 * D]), ("peer_subkeys", [16, 128, 128]), ("peer_u", [16384, D]),
                    ("peer_v", [16384, D]), ("final_norm_g", [D])]:
        io[nm] = din(nm, shp)
    io["abias"] = din("abias", [16, 128, 128])
    io["vpen"] = din("vpen", [16, 128, 16])
    io["notown"] = din("notown", [16, 128, 16])
    io["cpa"] = din("cpa", [2, 128, 256])
    io["cpb"] = din("cpb", [2, 128, 256])
    io["krow"] = din("krow", [1, 8 * 512])
    io["identf"] = din("identf", [128, 128])
    io["iota16"] = din("iota16", [128, 16])
    out = nc.dram_tensor("out", [NB_TOK, D], F32, kind="ExternalOutput").ap()

    def dscr(name, shape, dt):
        return nc.dram_tensor(name, list(shape), dt, kind=DBG_KIND.get(name, "Internal")).ap()

    kT_d = dscr("kT_d", [8, 128, SEQ], BF16)
    v_d = dscr("v_d", [SEQ, D], BF16)
    convo_d = dscr("convo_d", [D, NB_TOK], BF16)
    h1_d = dscr("h1_d", [NB_TOK, D], F32)
    h2_d = dscr("h2_d", [NB_TOK, D], F32)
    uv_d = dscr("uv_d", [16384, 2 * D], BF16)
    trk_ub = [Trk() for _ in range(8)]
    trk_vb = [Trk() for _ in range(8)]
    trk_kT = [Trk() for _ in range(8)]
    trk_v = [Trk() for _ in range(8)]
    trk_convo = [Trk() for _ in range(8)]
    trk_h1 = [Trk() for _ in range(16)]
    trk_h2 = [Trk() for _ in range(16)]
    trk_out = Trk()

    es = contextlib.ExitStack()
    with es:
        kb = KB(nc, es)
        pe, act, dve, pool, sp = kb.pe, kb.act, kb.dve, kb.pool, kb.sp
        dbg_trks = []

        def dbg(name, tile, shape, dt):
            if not DEBUG:
                return
            d = nc.dram_tensor("dbg_" + name, list(shape), dt, kind="ExternalOutput").ap()
            tk = Trk()
            src = tile if isinstance(tile, T) else tile[0]
            ap = tile[:] if isinstance(tile, T) else tile[1]
            kb.dma(sp, lambda: nc.sync.dma_start(out=d, in_=ap), r=[src], w=[tk])
            dbg_trks.append(tk)

        identf = kb.sb([128, 128], F32, name="identf")
        identb = kb.sb([128, 128], BF16, name="identb")
        kb.dma(sp, lambda: nc.sync.dma_start(out=identf[:], in_=io["identf"]), w=[identf])
        kb.dma(pool, lambda: nc.gpsimd.dma_start(out=identb[:], in_=io["identf"]), w=[identb])
        kmeanT = kb.sb([128, 8, 16], BF16, name="kmeanT")
        epsc = kb.sb([128, 1], F32, name="epsc")
        kb.op(dve, lambda: nc.vector.memset(epsc[:], EPS), w=[epsc])
        def load_w(dst, src_ap, cols, q=None):
            src = src_ap.rearrange("(dc p) n -> p dc n", p=128)[:, :, cols[0]:cols[1]]
            for dc in range(0, 8, 2):
                kb.dma(pool, (lambda dc=dc: nc.gpsimd.dma_start(out=dst[:, dc:dc + 2, :], in_=src[:, dc:dc + 2, :])),
                       w=[dst])

        def colvec(src_ap, es_, n=8):
            t = kb.sb([128, n], F32, es=es_, name="colv")
            kb.dma(sp, lambda: nc.sync.dma_start(out=t[:], in_=src_ap.rearrange("(c p) -> p c", p=128),
                                                 allow_slow_non_contiguous=True), w=[t])
            return t

        class NormT:
            def __init__(self, es_, nbuf=2, npsum=None):
                self.junk = [kb.sb([128, D], BF16, es=es_, name="junk") for _ in range(nbuf)]
                self.ss = [kb.sb([128, 1], F32, es=es_, name="ss") for _ in range(nbuf)]
                self.rs = [kb.sb([128, 1], F32, es=es_, name="rs") for _ in range(nbuf)]
                self.xnb = [kb.sb([128, D], BF16, es=es_, name="xnb") for _ in range(nbuf)]
                self.pT = [kb.ps([128, D], BF16, es=es_, name="pT") for _ in range(npsum or nbuf)]
                self.i = 0
                self.nbuf = nbuf
                self.es_ = es_
                self.g = {}

            def getg(self, nm):
                if nm not in self.g:
                    g = kb.sb([128, D], F32, es=self.es_, name="grep")
                    src = AP(io[nm], 0, [[0, 128], [1, D]])
                    kb.dma(sp, (lambda g=g, src=src: nc.sync.dma_start(out=g[:], in_=src)), w=[g])
                    self.g[nm] = g
                return self.g[nm]

            def run(self, xt, P, gname, dstT, col0, xn_f32=None, dst_trk=None):
                b = self.i % self.nbuf
                self.i += 1
                junk, ss, rs, xnb, pT = self.junk[b], self.ss[b], self.rs[b], self.xnb[b], self.pT[b % len(self.pT)]
                g = self.getg(gname)
                kb.op(act, lambda: nc.scalar.activation(out=junk[0:P, :], in_=xt[0:P, :], func=AF.Square,
                                                        accum_out=ss[0:P, :]), r=[xt], w=[junk, ss])
                kb.op(act, lambda: nc.scalar.activation(out=rs[0:P, :], in_=ss[0:P, :], func=AF.Sqrt, bias=epsc[0:P, :],
                                                        scale=1.0 / D), r=[ss, epsc], w=[rs])
                kb.op(dve, lambda: nc.vector.reciprocal(out=rs[0:P, :], in_=rs[0:P, :]), r=[rs], w=[rs])
                if xn_f32 is not None:
                    kb.op(dve, lambda: nc.vector.scalar_tensor_tensor(out=xn_f32[0:P, :], in0=xt[0:P, :], scalar=rs[0:P, :],
                                                                      in1=g[0:P, :], op0=ALU.mult, op1=ALU.mult),
                          r=[xt, rs, g], w=[xn_f32])
                    kb.op(pool, lambda: nc.gpsimd.tensor_copy(out=xnb[0:P, :], in_=xn_f32[0:P, :]), r=[xn_f32], w=[xnb])
                else:
                    kb.op(dve, lambda: nc.vector.scalar_tensor_tensor(out=xnb[0:P, :], in0=xt[0:P, :], scalar=rs[0:P, :],
                                                                      in1=g[0:P, :], op0=ALU.mult, op1=ALU.mult),
                          r=[xt, rs, g], w=[xnb])
                for dc in range(8):
                    kb.op(pe, (lambda dc=dc: nc.tensor.transpose(out=pT[:, dc * 128:dc * 128 + P],
                                                                 in_=xnb[0:P, dc * 128:(dc + 1) * 128],
                                                                 identity=identb[0:P, 0:P])), r=[xnb, identb], w=[pT])
                src = AP(pT[:], 0, [[D, 128], [128, 8], [1, P]])
                kb.op(act, lambda: nc.scalar.copy(out=dstT[:, :, col0:col0 + P], in_=src), r=[pT],
                      w=[dst_trk if dst_trk is not None else dstT])


        def phase0():
            kb.barrier()
            with contextlib.ExitStack() as e0:
                Wk = kb.sb([128, 8, D], BF16, es=e0, name="Wk")
                Wv = kb.sb([128, 8, D], BF16, es=e0, name="Wv")
                load_w(Wk, io["w_in"], (D, 2 * D))
                load_w(Wv, io["w_in"], (2 * D, 3 * D))
                cvt = [kb.sb([128, 16, D], BF16, es=e0, name="cvt") for _ in range(2)]
                ncv = 0
                for (src_t, off, trks) in ((io["peer_u"], 0, trk_ub), (io["peer_v"], D, trk_vb)):
                    sv = src_t.rearrange("(p j) d -> p j d", p=128)
                    dv = uv_d.rearrange("(p j) d -> p j d", p=128)[:, :, off:off + D]
                    for ch in range(8):
                        cb = cvt[ncv % 2]
                        ncv += 1
                        kb.dma(pool, (lambda cb=cb, sv=sv, ch=ch: nc.gpsimd.dma_start(out=cb[:], in_=sv[:, ch * 16:(ch + 1) * 16, :])),
                               w=[cb])
                        kb.dma(pool, (lambda cb=cb, dv=dv, ch=ch: nc.gpsimd.dma_start(out=dv[:, ch * 16:(ch + 1) * 16, :], in_=cb[:])),
                               r=[cb], w=[trks[ch]])
                nt = NormT(e0)
                xts = [kb.sb([128, D], F32, es=e0, name="xt") for _ in range(3)]
                xnT = [kb.sb([128, 8, 512], BF16, es=e0, name="xnT") for _ in range(2)]
                kTs = [kb.sb([128, 8, 512], BF16, es=e0, name="kTs") for _ in range(2)]
                vs = [kb.sb([128, 4, D], BF16, es=e0, name="vs") for _ in range(2)]
                psK = [kb.ps([128, 512], F32, es=e0, name="psK") for _ in range(2)]
                psV = [kb.ps([128, 512], F32, es=e0, name="psV") for _ in range(2)]
                kms = kb.sb([128, 8, 16], F32, es=e0, name="kms")
                xi = 0
                if STOP == 1: return
                for G in range(8):
                    xT = xnT[G % 2]
                    for tt in range(4):
                        xt = xts[xi % 3]
                        xi += 1
                        r0 = G * 512 + tt * 128
                        kb.dma(sp, (lambda xt=xt, r0=r0: nc.sync.dma_start(out=xt[:], in_=io["xfull"][r0:r0 + 128, :])), w=[xt])
                        nt.run(xt, 128, "mix_norm_g", xT, tt * 128)
                        if STOP == 2: return
                    if STOP == 3: return
                    kT = kTs[G % 2]
                    for h in range(8):
                        ps = psK[h % 2]
                        for dc in range(8):
                            kb.op(pe, (lambda dc=dc, h=h, ps=ps: nc.tensor.matmul(ps[:], lhsT=Wk[:, dc, h * 128:(h + 1) * 128],
                                                                                rhs=xT[:, dc, :], start=(dc == 0), stop=(dc == 7))),
                                  r=[Wk, xT], w=[ps])
                        for bb in range(2):
                            kb.op(act, (lambda h=h, ps=ps, bb=bb: nc.scalar.activation(
                                out=kT[:, h, bb * 256:(bb + 1) * 256], in_=ps[:, bb * 256:(bb + 1) * 256], func=AF.Copy,
                                accum_out=kms[:, h, G * 2 + bb:G * 2 + bb + 1])), r=[ps], w=[kT, kms])
                    if STOP in (4, 41): return
                    vv = vs[G % 2]
                    for tt in range(4):
                        for half in range(2):
                            ps = psV[half]
                            for dc in range(8):
                                kb.op(pe, (lambda dc=dc, tt=tt, half=half, ps=ps: nc.tensor.matmul(
                                    ps[:], lhsT=xT[:, dc, tt * 128:(tt + 1) * 128], rhs=Wv[:, dc, half * 512:(half + 1) * 512],
                                    start=(dc == 0), stop=(dc == 7))), r=[Wv, xT], w=[ps])
                            kb.op(dve, (lambda tt=tt, half=half, ps=ps: nc.vector.tensor_copy(
                                out=vv[:, tt, half * 512:(half + 1) * 512], in_=ps[:])), r=[ps], w=[vv])
                    if STOP == 5: return
                    kb.dma(sp, (lambda G=G, kT=kT: nc.sync.dma_start(
                        out=kT_d.rearrange("h p t -> p h t")[:, :, G * 512:(G + 1) * 512], in_=kT[:])), r=[kT], w=[trk_kT[G]])
                    kb.dma(sp, (lambda G=G, vv=vv: nc.sync.dma_start(
                        out=v_d.rearrange("(n p) c -> p n c", p=128)[:, G * 4:(G + 1) * 4, :], in_=vv[:])), r=[vv], w=[trk_v[G]])
                kb.op(act, lambda: nc.scalar.mul(out=kmeanT[:], in_=kms[:], mul=1.0 / 256.0), r=[kms], w=[kmeanT])

        if "p0" in PHASES:
            phase0()

        def phase1a():
            kb.barrier()
            with contextlib.ExitStack() as e1:
                Wa = kb.sb([128, 8, D], BF16, es=e1, name="Wa")
                Wb = kb.sb([128, 8, D], BF16, es=e1, name="Wb")
                Wco = kb.sb([128, 8, D], BF16, es=e1, name="Wco")
                load_w(Wa, io["w_in"], (3 * D, 4 * D))
                load_w(Wb, io["w_in"], (4 * D, 5 * D))
                load_w(Wco, io["w_conv_out"], (0, D))
                dwb = colvec(io["conv_dw_b"], e1)
                lng = colvec(io["conv_ln_g"], e1)
                lnb = colvec(io["conv_ln_b"], e1)
                bco = colvec(io["b_conv_out"], e1)
                wdw = kb.sb([128, 31, 8], F32, es=e1, name="wdw")
                for k0 in range(0, 31, 8):
                    k1 = min(31, k0 + 8)
                    kb.dma(sp, (lambda k0=k0, k1=k1: nc.sync.dma_start(
                        out=wdw[:, k0:k1, :], in_=io["conv_dw_w"][k0:k1, :].rearrange("k (c p) -> p k c", p=128),
                        allow_slow_non_contiguous=True)), w=[wdw])
                diag = kb.sb([128, 8, 31, 128], BF16, es=e1, name="diag")
                for cc in range(8):
                    for k in range(31):
                        if k % 2 == 0:
                            kb.op(dve, (lambda cc=cc, k=k: nc.vector.tensor_scalar(
                                out=diag[:, cc, k, :], in0=identf[:], scalar1=wdw[:, k, cc:cc + 1], scalar2=None, op0=ALU.mult)),
                                r=[identf, wdw], w=[diag.k((cc, k))])
                        else:
                            kb.op(act, (lambda cc=cc, k=k: nc.scalar.activation(
                                out=diag[:, cc, k, :], in_=identf[:], func=AF.Copy, scale=wdw[:, k, cc:cc + 1])),
                                r=[identf, wdw], w=[diag.k((cc, k))])
                onesm = kb.sb([128, 128], BF16, es=e1, name="onesm")
                kb.op(dve, lambda: nc.vector.memset(onesm[:], 1.0 / D), w=[onesm])
                nt = NormT(e1)
                xts = [kb.sb([128, D], F32, es=e1, name="xt") for _ in range(2)]
                xnT = [kb.sb([128, 8, 288], BF16, es=e1, name="xnT") for _ in range(2)]
                uT = [kb.sb([128, 8, 288], BF16, es=e1, name="uT") for _ in range(2)]
                sgb = [kb.sb([128, 288], F32, es=e1, name="sgb") for _ in range(2)]
                yT = kb.sb([128, 8, 256], F32, es=e1, name="yT")
                yb = kb.sb([128, 8, 256], BF16, es=e1, name="yb")
                ysq = kb.sb([128, 8, 256], BF16, es=e1, name="ysq")
                actT = kb.sb([128, 8, 256], BF16, es=e1, name="actT")
                coT = [kb.sb([128, 8, 256], BF16, es=e1, name="coT") for _ in range(1)]
                st = {n: kb.sb([128, 256], F32, es=e1, name=n) for n in ["m2", "var", "rstd", "nmr"]}
                t1 = [kb.sb([128, 256], F32, es=e1, name="t1") for _ in range(2)]
                mst = kb.sb([128, 512], F32, es=e1, name="mst")
                psA = [kb.ps([128, 512], F32, es=e1, name="psA") for _ in range(2)]
                psB = [kb.ps([128, 512], F32, es=e1, name="psB") for _ in range(2)]
                psC = [kb.ps([128, 512], F32, es=e1, name="psC") for _ in range(2)]
                xi = 0
                for i in range(8):
                    xT = xnT[i % 2]
                    for tt, P in enumerate([128, 128, 32]):
                        xt = xts[xi % 2]
                        xi += 1
                        kb.dma(sp, (lambda xt=xt, tt=tt, P=P, i=i: nc.sync.dma_start(
                            out=xt[0:P, :], in_=io["xconv"][i, tt * 128:tt * 128 + P, :])), w=[xt])
                        nt.run(xt, P, "mix_norm_g", xT, tt * 128)
                    u = uT[i % 2]
                    for cc in range(8):
                        pa, pb, sg = psA[cc % 2], psB[cc % 2], sgb[cc % 2]
                        for dc in range(8):
                            kb.op(pe, (lambda dc=dc, cc=cc, pa=pa: nc.tensor.matmul(
                                pa[:, 0:288], lhsT=Wa[:, dc, cc * 128:(cc + 1) * 128], rhs=xT[:, dc, :],
                                start=(dc == 0), stop=(dc == 7))), r=[Wa, xT], w=[pa])
                        for dc in range(8):
                            kb.op(pe, (lambda dc=dc, cc=cc, pb=pb: nc.tensor.matmul(
                                pb[:, 0:288], lhsT=Wb[:, dc, cc * 128:(cc + 1) * 128], rhs=xT[:, dc, :],
                                start=(dc == 0), stop=(dc == 7))), r=[Wb, xT], w=[pb])
                        kb.op(act, (lambda pb=pb, sg=sg: nc.scalar.activation(out=sg[:], in_=pb[:, 0:288], func=AF.Sigmoid)),
                              r=[pb], w=[sg])
                        kb.op(dve, (lambda pa=pa, sg=sg, cc=cc: nc.vector.tensor_tensor(
                            out=u[:, cc, :], in0=pa[:, 0:288], in1=sg[:], op=ALU.mult)), r=[pa, sg], w=[u])
                    for cc in range(8):
                        pc = psC[cc % 2]
                        for k in range(31):
                            kb.op(pe, (lambda cc=cc, k=k, pc=pc: nc.tensor.matmul(
                                pc[:, 0:256], lhsT=diag[:, cc, k, :], rhs=u[:, cc, 2 + k:2 + k + 256],
                                start=(k == 0), stop=(k == 30))), r=[diag.k((cc, k)), u], w=[pc])
                        kb.op(act, (lambda cc=cc, pc=pc: nc.scalar.activation(
                            out=yT[:, cc, :], in_=pc[:, 0:256], func=AF.Identity, bias=dwb[:, cc:cc + 1], scale=1.0)),
                            r=[pc, dwb], w=[yT.k(cc)])
                        kb.op(act, (lambda cc=cc, pc=pc: nc.scalar.activation(
                            out=ysq[:, cc, :], in_=pc[:, 0:256], func=AF.Square, bias=dwb[:, cc:cc + 1], scale=1.0)),
                            r=[pc, dwb], w=[ysq.k(cc)])
                        kb.op(act, (lambda cc=cc, pc=pc: nc.scalar.activation(
                            out=yb[:, cc, :], in_=pc[:, 0:256], func=AF.Identity, bias=dwb[:, cc:cc + 1], scale=1.0)),
                            r=[pc, dwb], w=[yb.k(cc)])
                    pst = psA[0]
                    for cc in range(8):
                        kb.op(pe, (lambda cc=cc: nc.tensor.matmul(pst[:, 0:256], lhsT=onesm[:], rhs=yb[:, cc, :],
                                                                 start=(cc == 0), stop=(cc == 7))), r=[onesm, yb.k(cc)], w=[pst])
                    for cc in range(8):
                        kb.op(pe, (lambda cc=cc: nc.tensor.matmul(pst[:, 256:512], lhsT=onesm[:], rhs=ysq[:, cc, :],
                                                                 start=(cc == 0), stop=(cc == 7))), r=[onesm, ysq.k(cc)], w=[pst])
                    m2, var, rstd, nmr = st["m2"], st["var"], st["rstd"], st["nmr"]
                    kb.op(act, lambda: nc.scalar.copy(out=mst[:], in_=pst[:]), r=[pst], w=[mst])
                    pst = mst
                    kb.op(dve, lambda: nc.vector.tensor_tensor(out=m2[:], in0=pst[:, 0:256], in1=pst[:, 0:256], op=ALU.mult),
                          r=[pst], w=[m2])
                    kb.op(dve, lambda: nc.vector.tensor_tensor(out=var[:], in0=pst[:, 256:512], in1=m2[:], op=ALU.subtract),
                          r=[pst, m2], w=[var])
                    kb.op(act, lambda: nc.scalar.activation(out=rstd[:], in_=var[:], func=AF.Sqrt, bias=epsc[:], scale=1.0),
                          r=[var, epsc], w=[rstd])
                    kb.op(dve, lambda: nc.vector.reciprocal(out=rstd[:], in_=rstd[:]), r=[rstd], w=[rstd])
                    kb.op(dve, lambda: nc.vector.scalar_tensor_tensor(out=nmr[:], in0=pst[:, 0:256], scalar=-1.0, in1=rstd[:],
                                                                      op0=ALU.mult, op1=ALU.mult), r=[pst, rstd], w=[nmr])
                    for cc in range(8):
                        tb = t1[cc % 2]
                        kb.op(dve, (lambda cc=cc, tb=tb: nc.vector.tensor_tensor(out=tb[:], in0=yT[:, cc, :], in1=rstd[:],
                                                                                op=ALU.mult)), r=[yT.k(cc), rstd], w=[tb])
                        kb.op(dve, (lambda cc=cc, tb=tb: nc.vector.tensor_tensor(out=tb[:], in0=tb[:], in1=nmr[:], op=ALU.add)),
                              r=[tb, nmr], w=[tb])
                        kb.op(act, (lambda cc=cc, tb=tb: nc.scalar.activation(
                            out=actT[:, cc, :], in_=tb[:], func=AF.Silu, bias=lnb[:, cc:cc + 1], scale=lng[:, cc:cc + 1])),
                            r=[tb, lnb, lng], w=[actT.k(cc)])
                    co = coT[0]
                    for nn in range(8):
                        po = psB[nn % 2]
                        for cc in range(8):
                            kb.op(pe, (lambda cc=cc, nn=nn, po=po: nc.tensor.matmul(
                                po[:, 0:256], lhsT=Wco[:, cc, nn * 128:(nn + 1) * 128], rhs=actT[:, cc, :],
                                start=(cc == 0), stop=(cc == 7))), r=[Wco, actT.k(cc)], w=[po])
                        kb.op(act, (lambda nn=nn, po=po: nc.scalar.activation(
                            out=co[:, nn, :], in_=po[:, 0:256], func=AF.Identity, bias=bco[:, nn:nn + 1], scale=1.0)),
                            r=[po, bco], w=[co])
                    kb.dma(sp, (lambda i=i, co=co: nc.sync.dma_start(
                        out=convo_d.rearrange("(nn p) t -> p nn t", p=128)[:, :, i * 256:(i + 1) * 256], in_=co[:])),
                        r=[co], w=[trk_convo[i]])


        if "p1a" in PHASES:
            phase1a()

        def phase1b():
            kb.barrier()
            with contextlib.ExitStack() as e2:
                Wq = kb.sb([128, 8, D], BF16, es=e2, name="Wq")
                Wgc = kb.sb([128, 8, D], BF16, es=e2, name="Wgc")
                Wga = kb.sb([128, 8, D], BF16, es=e2, name="Wga")
                Wao = kb.sb([128, 8, D], BF16, es=e2, name="Wao")
                Wmx = kb.sb([128, 8, D], BF16, es=e2, name="Wmx")
                load_w(Wq, io["w_in"], (0, D))
                load_w(Wgc, io["w_in"], (5 * D, 6 * D))
                load_w(Wga, io["w_in"], (6 * D, 7 * D))
                load_w(Wao, io["w_attn_out"], (0, D))
                load_w(Wmx, io["w_mix_out"], (0, D))
                krow = kb.sb([128, 8 * 512], BF16, es=e2, name="krow")
                kb.op(dve, lambda: nc.vector.memset(krow[:], 0.0), w=[krow])
                kb.dma(pool, lambda: nc.gpsimd.dma_start(out=krow[0:1, :], in_=io["krow"]), w=[krow])
                ones1 = kb.sb([128, 128], BF16, es=e2, name="ones1")
                kb.op(dve, lambda: nc.vector.memset(ones1[:], 0.0), w=[ones1])
                kb.op(dve, lambda: nc.vector.memset(ones1[0:1, :], 1.0), w=[ones1])
                cpab = kb.sb([128, 2, 512], BF16, es=e2, name="cpab")
                kb.dma(pool, lambda: nc.gpsimd.dma_start(out=cpab[:, :, 0:256], in_=io["cpa"].rearrange("s p k -> p s k")), w=[cpab])
                kb.dma(pool, lambda: nc.gpsimd.dma_start(out=cpab[:, :, 256:512], in_=io["cpb"].rearrange("s p k -> p s k")), w=[cpab])
                nt = NormT(e2, nbuf=2, npsum=1)
                xts = [kb.sb([128, D], F32, es=e2, name="xt") for _ in range(2)]
                xnT = kb.sb([128, 8, 256], BF16, es=e2, name="xnT")
                qT = kb.sb([128, 8, 256], BF16, es=e2, name="qT")
                sgc = kb.sb([128, 8, 256], BF16, es=e2, name="sgc")
                sga = kb.sb([128, 8, 256], BF16, es=e2, name="sga")
                coT = kb.sb([128, 8, 256], BF16, es=e2, name="coT")
                abias = kb.sb([128, 128], F32, es=e2, name="abias")
                vpen = kb.sb([128, 16], F32, es=e2, name="vpen")
                notown = kb.sb([128, 16], F32, es=e2, name="notown")
                gm = kb.sb([128, 8, 16], F32, es=e2, name="gm")
                mx8 = kb.sb([128, 8, 8], F32, es=e2, name="mx8")
                thr = kb.sb([128, 8], F32, es=e2, name="thr")
                sel = kb.sb([128, 8, 16], F32, es=e2, name="sel")
                biasall = [kb.sb([128, 8, 16], F32, es=e2, name="biasall") for _ in range(2)]
                kbuf = [kb.sb([128, 4096], BF16, es=e2, name="kbuf") for _ in range(2)]
                vbuf = [kb.sb([128, 32, 128], BF16, es=e2, name="vbuf") for _ in range(2)]
                Pb = [kb.sb([128, 4096], BF16, es=e2, name="Pb") for _ in range(1)]
                PT = [kb.sb([128, 32, 128], BF16, es=e2, name="PT") for _ in range(2)]
                zpart = [kb.sb([128, 16], F32, es=e2, name="zpart") for _ in range(2)]
                zs = [kb.sb([128, 1], F32, es=e2, name="zs") for _ in range(2)]
                zjunk = kb.sb([128, 16], F32, es=e2, name="zjunk")
                attn_tok = [kb.sb([128, D], BF16, es=e2, name="attn_tok") for _ in range(2)]
                attnT = kb.sb([128, 8, 256], BF16, es=e2, name="attnT")
                m1 = kb.sb([128, 256], F32, es=e2, name="m1")
                m2b = kb.sb([128, 256], F32, es=e2, name="m2b")
                merged = kb.sb([128, 8, 256], BF16, es=e2, name="merged")
                psP = [kb.ps([128, 512], F32, es=e2, name="psP") for _ in range(2)]
                psS = [kb.ps([128, 512], F32, es=e2, name="psS") for _ in range(2)]
                psT = [kb.ps([128, 512], BF16, es=e2, name="psT") for _ in range(2)]
                psO = kb.ps([128, 512], F32, es=e2, name="psO")
                pcnt = [0]

                def nextP():
                    pcnt[0] += 1
                    return psP[pcnt[0] % 2]

                for i in range(8):
                    NBK = 2 * i + 2
                    for s_ in range(2):
                        xt = xts[s_]
                        kb.dma(sp, (lambda xt=xt, s_=s_, i=i: nc.sync.dma_start(
                            out=xt[:], in_=io["xown"][i * 256 + s_ * 128:i * 256 + (s_ + 1) * 128, :])), w=[xt])
                        nt.run(xt, 128, "mix_norm_g", xnT, s_ * 128)
                    kb.dma(sp, (lambda i=i: nc.sync.dma_start(
                        out=coT[:], in_=convo_d.rearrange("(nn p) t -> p nn t", p=128)[:, :, i * 256:(i + 1) * 256])),
                        r=[trk_convo[i]], w=[coT])
                    for h in range(8):
                        ps = nextP()
                        for dc in range(8):
                            kb.op(pe, (lambda dc=dc, h=h, ps=ps: nc.tensor.matmul(
                                ps[:, 0:256], lhsT=Wq[:, dc, h * 128:(h + 1) * 128], rhs=xnT[:, dc, :],
                                start=(dc == 0), stop=(dc == 7))), r=[Wq, xnT], w=[ps])
                        kb.op(act, (lambda h=h, ps=ps: nc.scalar.mul(out=qT[:, h, :], in_=ps[:, 0:256], mul=128.0 ** -0.5)),
                              r=[ps], w=[qT])
                    for (Wg, sg) in ((Wgc, sgc), (Wga, sga)):
                        for nn in range(8):
                            ps = nextP()
                            for dc in range(8):
                                kb.op(pe, (lambda dc=dc, nn=nn, ps=ps, Wg=Wg: nc.tensor.matmul(
                                    ps[:, 0:256], lhsT=Wg[:, dc, nn * 128:(nn + 1) * 128], rhs=xnT[:, dc, :],
                                    start=(dc == 0), stop=(dc == 7))), r=[Wg, xnT], w=[ps])
                            kb.op(act, (lambda nn=nn, ps=ps, sg=sg: nc.scalar.activation(
                                out=sg[:, nn, :], in_=ps[:, 0:256], func=AF.Sigmoid)), r=[ps], w=[sg])
                    if STOP == 10: return
                    for s_ in range(2):
                        c = 2 * i + s_
                        qs = slice(s_ * 128, (s_ + 1) * 128)
                        kb.dma(sp, (lambda c=c: nc.sync.dma_start(out=abias[:], in_=io["abias"][c])), w=[abias])
                        kb.dma(sp, (lambda c=c: nc.sync.dma_start(out=vpen[:], in_=io["vpen"][c])), w=[vpen])
                        kb.dma(sp, (lambda c=c: nc.sync.dma_start(out=notown[:], in_=io["notown"][c])), w=[notown])
                        psg = nextP()
                        for h in range(8):
                            kb.op(pe, (lambda h=h, psg=psg, qs=qs: nc.tensor.matmul(
                                psg[:, h * 16:(h + 1) * 16], lhsT=qT[:, h, qs], rhs=kmeanT[:, h, :], start=True, stop=True)),
                                r=[qT, kmeanT], w=[psg])
                        kb.op(dve, (lambda psg=psg: nc.vector.tensor_tensor(
                            out=gm[:], in0=AP(psg[:], 0, [[512, 128], [16, 8], [1, 16]]),
                            in1=AP(vpen[:], 0, [[16, 128], [0, 8], [1, 16]]), op=ALU.add)), r=[psg, vpen], w=[gm])
                        for h in range(8):
                            kb.op(dve, (lambda h=h: nc.vector.max(out=mx8[:, h, :], in_=gm[:, h, :])), r=[gm], w=[mx8])
                        kb.op(dve, lambda: nc.vector.tensor_scalar(out=thr[:], in0=mx8[:, :, 2], scalar1=-1e29, scalar2=None,
                                                                   op0=ALU.max), r=[mx8], w=[thr])
                        kb.op(dve, lambda: nc.vector.tensor_tensor(
                            out=sel[:], in0=gm[:], in1=AP(thr[:], 0, [[8, 128], [1, 8], [0, 16]]), op=ALU.is_ge),
                            r=[gm, thr], w=[sel])
                        kb.op(dve, lambda: nc.vector.tensor_scalar(out=sel[:], in0=sel[:], scalar1=-1.0, scalar2=-NEG,
                                                                   op0=ALU.add, op1=ALU.mult), r=[sel], w=[sel])
                        kb.op(dve, lambda: nc.vector.tensor_tensor(
                            out=sel[:], in0=sel[:], in1=AP(notown[:], 0, [[16, 128], [0, 8], [1, 16]]), op=ALU.mult),
                            r=[sel, notown], w=[sel])
                        ba = biasall[s_]
                        kb.op(dve, (lambda ba=ba: nc.vector.tensor_tensor(
                            out=ba[:], in0=sel[:], in1=AP(abias[:], 0, [[128, 128], [16, 8], [1, 16]]), op=ALU.add)),
                            r=[sel, abias], w=[ba])
                    if STOP == 11: return
                    for h in range(8):
                        kbf, vbf = kbuf[h % 2], vbuf[h % 2]
                        kb.dma(sp, (lambda h=h, kbf=kbf, NBK=NBK: nc.sync.dma_start(
                            out=kbf[:, 0:NBK * 256], in_=kT_d[h, :, 0:NBK * 256])), r=trk_kT[0:(NBK * 256 + 511) // 512], w=[kbf])
                        kb.dma(sp, (lambda h=h, vbf=vbf, NBK=NBK: nc.sync.dma_start(
                            out=vbf[:, 0:NBK * 2, :],
                            in_=v_d.rearrange("(n p) c -> p n c", p=128)[:, 0:NBK * 2, h * 128:(h + 1) * 128])),
                            r=trk_v[0:(NBK * 256 + 511) // 512], w=[vbf])
                        for s_ in range(2):
                            qs = slice(s_ * 128, (s_ + 1) * 128)
                            ba = biasall[s_]
                            P_, PT_, zp, z1 = Pb[0], PT[s_], zpart[s_], zs[s_]
                            for grp in range(i + 1):
                                ps = psS[grp % 2]
                                last = (grp == i)
                                kb.op(pe, (lambda ps=ps, grp=grp, qs=qs, h=h: nc.tensor.matmul(
                                    ps[:], lhsT=qT[:, h, qs], rhs=kbf[:, grp * 512:(grp + 1) * 512], start=True, stop=False)),
                                    r=[qT, kbf], w=[ps])
                                kb.op(pe, (lambda ps=ps, h=h, last=last: nc.tensor.matmul(
                                    ps[:], lhsT=ones1[:], rhs=krow[:, h * 512:(h + 1) * 512], start=False, stop=(not last))),
                                    r=[ones1, krow], w=[ps])
                                if last:
                                    kb.op(pe, (lambda ps=ps, s_=s_: nc.tensor.matmul(
                                        ps[:], lhsT=identb[:], rhs=cpab[:, s_, :], start=False, stop=True)),
                                        r=[identb, cpab], w=[ps])
                                for bb in range(2):
                                    n = grp * 2 + bb
                                    kb.op(act, (lambda ps=ps, bb=bb, n=n, h=h, ba=ba, P_=P_, zp=zp: nc.scalar.activation(
                                        out=P_[:, n * 256:(n + 1) * 256], in_=ps[:, bb * 256:(bb + 1) * 256], func=AF.Exp,
                                        bias=ba[:, h, n:n + 1], scale=1.0, accum_out=zp[:, n:n + 1])),
                                        r=[ps, ba], w=[P_.k(n), zp.k(n)])
                            kb.op(act, (lambda zp=zp, z1=z1, NBK=NBK: nc.scalar.activation(
                                out=zjunk[:, 0:NBK], in_=zp[:, 0:NBK], func=AF.Copy, accum_out=z1[:])),
                                r=[zp.k(n_) for n_ in range(NBK)], w=[z1, zjunk])
                            kb.op(dve, (lambda z1=z1: nc.vector.reciprocal(out=z1[:], in_=z1[:])), r=[z1], w=[z1])
                            if STOP == 12:
                                dbg("biasall", ba, [128, 8, 16], F32)
                                dbg("gm", gm, [128, 8, 16], F32)
                                dbg("sel", sel, [128, 8, 16], F32)
                                dbg("mx8", mx8, [128, 8, 8], F32)
                                dbg("zp", zp, [128, 16], F32)
                                dbg("z1", z1, [128, 1], F32)
                                dbg("P", P_, [128, 4096], BF16)
                                dbg("qT", qT, [128, 8, 256], BF16)
                                dbg("kbf", kbf, [128, 4096], BF16)
                                return
                            nkt = NBK * 2
                            for k0 in range(0, nkt, 4):
                                pt = psT[(k0 // 4) % 2]
                                for kt in range(k0, k0 + 4):
                                    kb.op(pe, (lambda pt=pt, kt=kt, k0=k0, P_=P_: nc.tensor.transpose(
                                        out=pt[:, (kt - k0) * 128:(kt - k0 + 1) * 128], in_=P_[:, kt * 128:(kt + 1) * 128],
                                        identity=identb[:])), r=[P_.k(kt // 2), identb], w=[pt])
                                kb.op(dve, (lambda pt=pt, k0=k0, PT_=PT_: nc.vector.tensor_copy(
                                    out=PT_[:, k0:k0 + 4, :], in_=AP(pt[:], 0, [[512, 128], [128, 4], [1, 128]]))),
                                    r=[pt], w=[PT_.k(k0 // 4)])
                            po = psO[:, (h % 4) * 128:(h % 4 + 1) * 128]
                            for kt in range(nkt):
                                kb.op(pe, (lambda kt=kt, po=po, PT_=PT_, nkt=nkt: nc.tensor.matmul(
                                    po, lhsT=PT_[:, kt, :], rhs=vbf[:, kt, :], start=(kt == 0), stop=(kt == nkt - 1))),
                                    r=[PT_.k(kt // 4), vbf], w=[psO])
                            at = attn_tok[s_]
                            kb.op(act, (lambda po=po, at=at, h=h, z1=z1: nc.scalar.activation(
                                out=at[:, h * 128:(h + 1) * 128], in_=po, func=AF.Copy, scale=z1[:, 0:1])),
                                r=[psO, z1], w=[at])
                            if STOP == 13:
                                dbg("at", at, [128, 1024], BF16)
                                dbg("PT", PT_, [128, 32, 128], BF16)
                                dbg("vbf", vbf, [128, 32, 128], BF16)
                                dbg("z1", z1, [128, 1], F32)
                                dbg("P", P_, [128, 4096], BF16)
                                return
                    if STOP == 14: return
                    for s_ in range(2):
                        at = attn_tok[s_]
                        for k0 in range(0, 8, 4):
                            pt = psT[(k0 // 4) % 2]
                            for hc in range(k0, k0 + 4):
                                kb.op(pe, (lambda pt=pt, hc=hc, k0=k0, at=at: nc.tensor.transpose(
                                    out=pt[:, (hc - k0) * 128:(hc - k0 + 1) * 128], in_=at[:, hc * 128:(hc + 1) * 128],
                                    identity=identb[:])), r=[at, identb], w=[pt])
                            kb.op(dve, (lambda pt=pt, k0=k0, s_=s_: nc.vector.tensor_copy(
                                out=attnT[:, k0:k0 + 4, s_ * 128:(s_ + 1) * 128],
                                in_=AP(pt[:], 0, [[512, 128], [128, 4], [1, 128]]))), r=[pt], w=[attnT])
                    for nn in range(8):
                        ps = nextP()
                        for hc in range(8):
                            kb.op(pe, (lambda hc=hc, nn=nn, ps=ps: nc.tensor.matmul(
                                ps[:, 0:256], lhsT=Wao[:, hc, nn * 128:(nn + 1) * 128], rhs=attnT[:, hc, :],
                                start=(hc == 0), stop=(hc == 7))), r=[Wao, attnT], w=[ps])
                        kb.op(dve, (lambda nn=nn, ps=ps: nc.vector.tensor_tensor(out=m1[:], in0=ps[:, 0:256], in1=sga[:, nn, :],
                                                                                op=ALU.mult)), r=[ps, sga], w=[m1])
                        kb.op(dve, (lambda nn=nn: nc.vector.tensor_tensor(out=m2b[:], in0=coT[:, nn, :], in1=sgc[:, nn, :],
                                                                          op=ALU.mult)), r=[coT, sgc], w=[m2b])
                        kb.op(dve, (lambda nn=nn: nc.vector.tensor_tensor(out=merged[:, nn, :], in0=m1[:], in1=m2b[:], op=ALU.add)),
                              r=[m1, m2b], w=[merged])
                    if STOP == 15:
                        dbg("merged", merged, [128, 8, 256], BF16)
                        dbg("attnT", attnT, [128, 8, 256], BF16)
                        dbg("sga", sga, [128, 8, 256], BF16)
                        dbg("sgc", sgc, [128, 8, 256], BF16)
                        dbg("coT", coT, [128, 8, 256], BF16)
                        dbg("at0", attn_tok[0], [128, 1024], BF16)
                        dbg("at1", attn_tok[1], [128, 1024], BF16)
                        return
                    for s_ in range(2):
                        ht = xts[s_]
                        for half in range(2):
                            ps = nextP()
                            for nn in range(8):
                                kb.op(pe, (lambda nn=nn, ps=ps, s_=s_, half=half: nc.tensor.matmul(
                                    ps[:], lhsT=merged[:, nn, s_ * 128:(s_ + 1) * 128], rhs=Wmx[:, nn, half * 512:(half + 1) * 512],
                                    start=(nn == 0), stop=(nn == 7))), r=[merged, Wmx], w=[ps])
                            kb.op(dve, (lambda ps=ps, s_=s_, half=half, ht=ht: nc.vector.tensor_tensor(
                                out=ht[:, half * 512:(half + 1) * 512], in0=ps[:], in1=xts[s_][:, half * 512:(half + 1) * 512],
                                op=ALU.add)), r=[ps, xts[s_]], w=[ht])
                        kb.dma(sp, (lambda ht=ht, s_=s_, i=i: nc.sync.dma_start(
                            out=h1_d[i * 256 + s_ * 128:i * 256 + (s_ + 1) * 128, :], in_=ht[:])), r=[ht], w=[trk_h1[2 * i + s_]])

        if "p1b" in PHASES:
            phase1b()


        def phase2():
            kb.barrier()
            with contextlib.ExitStack() as e3:
                Wxq = kb.sb([128, 8, D], BF16, es=e3, name="Wxq")
                Wxo = kb.sb([128, 8, D], BF16, es=e3, name="Wxo")
                Wkv = kb.sb([128, 8, 2 * D], BF16, es=e3, name="Wkv")
                load_w(Wxq, io["w_xq"], (0, D))
                load_w(Wxo, io["w_xo"], (0, D))
                load_w(Wkv, io["w_xkv"], (0, 2 * D))
                nt = NormT(e3, nbuf=2)
                xts = [kb.sb([128, D], F32, es=e3, name="xt") for _ in range(4)]
                mnT = kb.sb([128, 8, 256], BF16, es=e3, name="mnT")
                kxT = kb.sb([128, 8, 256], BF16, es=e3, name="kxT")
                vx = kb.sb([128, 2, D], BF16, es=e3, name="vx")
                hnT = kb.sb([128, 8, 512], BF16, es=e3, name="hnT")
                qxT = kb.sb([128, 8, 512], BF16, es=e3, name="qxT")
                Px = kb.sb([128, 4, 256], BF16, es=e3, name="Px")
                PxT = kb.sb([128, 8, 128], BF16, es=e3, name="PxT")
                zx = kb.sb([128, 4], F32, es=e3, name="zx")
                o_tok = kb.sb([128, D], BF16, es=e3, name="o_tok")
                oT = kb.sb([128, 8, 128], BF16, es=e3, name="oT")
                psP = [kb.ps([128, 512], F32, es=e3, name="psP") for _ in range(2)]
                psS = [kb.ps([128, 512], F32, es=e3, name="psS") for _ in range(2)]
                psT = kb.ps([128, D], BF16, es=e3, name="psT")
                psO = kb.ps([128, 512], F32, es=e3, name="psO")
                for mt in range(2):
                    xt = xts[mt]
                    kb.dma(sp, (lambda xt=xt, mt=mt: nc.sync.dma_start(out=xt[:], in_=io["mem"][mt * 128:(mt + 1) * 128, :])), w=[xt])
                    nt.run(xt, 128, "mem_norm_g", mnT, mt * 128)
                for c in range(8):
                    ps = psP[c % 2]
                    for dc in range(8):
                        kb.op(pe, (lambda dc=dc, c=c, ps=ps: nc.tensor.matmul(
                            ps[:, 0:256], lhsT=Wkv[:, dc, c * 128:(c + 1) * 128], rhs=mnT[:, dc, :],
                            start=(dc == 0), stop=(dc == 7))), r=[Wkv, mnT], w=[ps])
                    kb.op(act, (lambda c=c, ps=ps: nc.scalar.copy(out=kxT[:, c, :], in_=ps[:, 0:256])), r=[ps], w=[kxT])
                for mt in range(2):
                    for half in range(2):
                        ps = psP[half]
                        for dc in range(8):
                            kb.op(pe, (lambda dc=dc, mt=mt, half=half, ps=ps: nc.tensor.matmul(
                                ps[:], lhsT=mnT[:, dc, mt * 128:(mt + 1) * 128], rhs=Wkv[:, dc, D + half * 512:D + (half + 1) * 512],
                                start=(dc == 0), stop=(dc == 7))), r=[Wkv, mnT], w=[ps])
                        kb.op(dve, (lambda mt=mt, half=half, ps=ps: nc.vector.tensor_copy(
                            out=vx[:, mt, half * 512:(half + 1) * 512], in_=ps[:])), r=[ps], w=[vx])
                for G in range(4):
                    for tt in range(4):
                        xt = xts[tt]
                        ti = G * 4 + tt
                        kb.dma(sp, (lambda xt=xt, ti=ti: nc.sync.dma_start(out=xt[:], in_=h1_d[ti * 128:(ti + 1) * 128, :])),
                               r=[trk_h1[ti]], w=[xt])
                        nt.run(xt, 128, "xattn_norm_g", hnT, tt * 128)
                    for c in range(8):
                        ps = psP[c % 2]
                        for dc in range(8):
                            kb.op(pe, (lambda dc=dc, c=c, ps=ps: nc.tensor.matmul(
                                ps[:], lhsT=Wxq[:, dc, c * 128:(c + 1) * 128], rhs=hnT[:, dc, :],
                                start=(dc == 0), stop=(dc == 7))), r=[Wxq, hnT], w=[ps])
                        kb.op(act, (lambda c=c, ps=ps: nc.scalar.mul(out=qxT[:, c, :], in_=ps[:], mul=1.0 / 16.0)), r=[ps], w=[qxT])
                    for tt in range(4):
                        ti = G * 4 + tt
                        xt = xts[tt]
                        ts_ = slice(tt * 128, (tt + 1) * 128)
                        for hx in range(4):
                            ps = psS[hx // 2]
                            for j in range(2):
                                kb.op(pe, (lambda ps=ps, hx=hx, j=j, ts_=ts_: nc.tensor.matmul(
                                    ps[:, (hx % 2) * 256:(hx % 2 + 1) * 256], lhsT=qxT[:, hx * 2 + j, ts_], rhs=kxT[:, hx * 2 + j, :],
                                    start=(j == 0), stop=(j == 1))), r=[qxT, kxT], w=[ps])
                            kb.op(act, (lambda ps=ps, hx=hx: nc.scalar.activation(
                                out=Px[:, hx, :], in_=ps[:, (hx % 2) * 256:(hx % 2 + 1) * 256], func=AF.Exp,
                                accum_out=zx[:, hx:hx + 1])), r=[ps], w=[Px.k(hx), zx.k(hx)])
                        for hx in range(4):
                            for mt in range(2):
                                kb.op(pe, (lambda hx=hx, mt=mt: nc.tensor.transpose(
                                    out=psT[:, (hx * 2 + mt) * 128:(hx * 2 + mt + 1) * 128], in_=Px[:, hx, mt * 128:(mt + 1) * 128],
                                    identity=identb[:])), r=[Px.k(hx), identb], w=[psT])
                        kb.op(dve, lambda: nc.vector.tensor_copy(out=PxT[:], in_=AP(psT[:], 0, [[D, 128], [128, 8], [1, 128]])),
                              r=[psT], w=[PxT])
                        kb.op(dve, lambda: nc.vector.reciprocal(out=zx[:], in_=zx[:]), r=[zx.k(hx_) for hx_ in range(4)],
                              w=[zx.k(hx_) for hx_ in range(4)])
                        for hh in range(2):
                            for hx in (2 * hh, 2 * hh + 1):
                                for mt in range(2):
                                    kb.op(pe, (lambda hx=hx, mt=mt: nc.tensor.matmul(
                                        psO[:, (hx % 2) * 256:(hx % 2 + 1) * 256], lhsT=PxT[:, hx * 2 + mt, :],
                                        rhs=vx[:, mt, hx * 256:(hx + 1) * 256], start=(mt == 0), stop=(mt == 1))),
                                        r=[PxT, vx], w=[psO])
                                kb.op(act, (lambda hx=hx: nc.scalar.activation(
                                    out=o_tok[:, hx * 256:(hx + 1) * 256], in_=psO[:, (hx % 2) * 256:(hx % 2 + 1) * 256],
                                    func=AF.Copy, scale=zx[:, hx:hx + 1])), r=[psO, zx.k(hx)], w=[o_tok])
                        for c in range(8):
                            kb.op(pe, (lambda c=c: nc.tensor.transpose(out=psT[:, c * 128:(c + 1) * 128],
                                                                       in_=o_tok[:, c * 128:(c + 1) * 128], identity=identb[:])),
                                  r=[o_tok, identb], w=[psT])
                        kb.op(dve, lambda: nc.vector.tensor_copy(out=oT[:], in_=AP(psT[:], 0, [[D, 128], [128, 8], [1, 128]])),
                              r=[psT], w=[oT])
                        for half in range(2):
                            ps = psP[half]
                            for c in range(8):
                                kb.op(pe, (lambda c=c, half=half, ps=ps: nc.tensor.matmul(
                                    ps[:], lhsT=oT[:, c, :], rhs=Wxo[:, c, half * 512:(half + 1) * 512],
                                    start=(c == 0), stop=(c == 7))), r=[oT, Wxo], w=[ps])
                            kb.op(dve, (lambda half=half, ps=ps, xt=xt: nc.vector.tensor_tensor(
                                out=xt[:, half * 512:(half + 1) * 512], in0=ps[:], in1=xt[:, half * 512:(half + 1) * 512],
                                op=ALU.add)), r=[ps, xt], w=[xt])
                        kb.dma(sp, (lambda xt=xt, ti=ti: nc.sync.dma_start(out=h2_d[ti * 128:(ti + 1) * 128, :], in_=xt[:])),
                               r=[xt], w=[trk_h2[ti]])

        if "p2" in PHASES:
            phase2()


        def phase3():
            kb.barrier()
            with contextlib.ExitStack() as e4:
                Wpq = kb.sb([128, 8, 2 * D], BF16, es=e4, name="Wpq")
                load_w(Wpq, io["w_peer_q"], (0, 2 * D))
                nt = NormT(e4, nbuf=2, npsum=1)
                gfin = nt.getg("final_norm_g")
                keysT = kb.sb([128, 16, 128], BF16, es=e4, name="keysT")
                ktmp = [kb.sb([128, 128], BF16, es=e4, name="ktmp") for _ in range(2)]
                xts = [kb.sb([128, D], F32, es=e4, name="xt") for _ in range(2)]
                xpTs = [kb.sb([128, 8, 128], BF16, es=e4, name="xpT") for _ in range(2)]
                qpT = kb.sb([128, 16, 128], BF16, es=e4, name="qpT")
                s_sb = kb.sb([128, 16, 128], F32, es=e4, name="s_sb")
                s2_sb = kb.sb([128, 16, 128], F32, es=e4, name="s2_sb")
                v16 = kb.sb([128, 16, 16], F32, es=e4, name="v16")
                ix16 = kb.sb([128, 16, 16], U32, es=e4, name="ix16")
                cand = kb.sb([128, 8, 256], F32, es=e4, name="cand")
                cand2 = kb.sb([128, 8, 256], F32, es=e4, name="cand2")
                sc = kb.sb([128, 8, 16], F32, es=e4, name="sc")
                ci = kb.sb([128, 8, 16], U32, es=e4, name="ci")
                abu = kb.sb([128, 2, 128], U32, es=e4, name="abu")
                abf = kb.sb([128, 2, 128], F32, es=e4, name="abf")
                i12f = kb.sb([128, 2, 128], F32, es=e4, name="i12f")
                eq = kb.sb([128, 8, 16, 16], F32, es=e4, name="eq")
                i12b = kb.sb([128, 2, 128], BF16, es=e4, name="i12b")
                i12T = kb.sb([128, 2, 128], F32, es=e4, name="i12T")
                eTf = kb.sb([128, 128], F32, es=e4, name="eTf")
                eTi = [kb.sb([128, 128], I32, es=e4, name="eTi") for _ in range(2)]
                dsc = kb.sb([128, 8, 16], F32, es=e4, name="dsc")
                ex = kb.sb([128, 8, 16], F32, es=e4, name="ex")
                zsum = kb.sb([128, 8], F32, es=e4, name="zsum")
                gw = kb.sb([128, 8, 16], F32, es=e4, name="gw")
                gwT = [kb.sb([128, 128], F32, es=e4, name="gwT") for _ in range(2)]
                iota = kb.sb([128, 16], F32, es=e4, name="iota")
                kb.dma(sp, lambda: nc.sync.dma_start(out=iota[:], in_=io["iota16"]), w=[iota])
                uvr = [kb.sb([128, 2 * D], BF16, es=e4, name="uvr") for _ in range(16)]
                zg = [kb.sb([128, 8, 128], BF16, es=e4, name="zg") for _ in range(2)]
                wband = kb.sb([128, 8, 248], BF16, es=e4, name="wband")
                kb.op(dve, lambda: nc.vector.memset(wband[:], 0.0), w=[wband])
                for j_ in range(8):
                    kb.op(dve, (lambda j_=j_: nc.vector.memset(wband[:, j_, 120 + j_:121 + j_], 1.0)), w=[wband])
                junk5 = kb.sb([128, D], BF16, es=e4, name="junk5")
                adot = kb.sb([128, 128, 2], F32, es=e4, name="adot")
                asum = kb.sb([128, 8], F32, es=e4, name="asum")
                agl = kb.sb([128, 8], F32, es=e4, name="agl")
                cT = [kb.sb([128, 8], BF16, es=e4, name="cT") for _ in range(2)]
                ssf = kb.sb([128, 1], F32, es=e4, name="ssf")
                rsf = kb.sb([128, 1], F32, es=e4, name="rsf")
                psA1 = kb.ps([128, 512], F32, es=e4, name="psA")
                psA = [psA1, psA1]
                psTb = nt.pT[0]
                psU = [kb.ps([128, D], BF16, es=e4, name="psU") for _ in range(2)]
                psAd = kb.ps([128, 512], F32, es=e4, name="psAd")
                UTs = [kb.sb([128, 8, 128], BF16, es=e4, name="UTs") for _ in range(3)]
                psY = kb.ps([128, D], F32, es=e4, name="psY")
                for c in range(16):
                    kt_ = ktmp[c % 2]
                    kb.dma(pool, (lambda c=c, kt_=kt_: nc.gpsimd.dma_start(out=kt_[:], in_=io["peer_subkeys"][c])), w=[kt_])
                    kb.op(pe, (lambda c=c, kt_=kt_: nc.tensor.transpose(out=psTb[:, (c % 4) * 128:(c % 4 + 1) * 128], in_=kt_[:],
                                                                        identity=identb[:])), r=[kt_, identb], w=[psTb])
                    if c % 4 == 3:
                        kb.op(dve, (lambda c=c: nc.vector.tensor_copy(
                            out=keysT[:, c - 3:c + 1, :], in_=AP(psTb[:], 0, [[D, 128], [128, 4], [1, 128]]))), r=[psTb], w=[keysT])
                gi_box = [0]

                def prologue(ti):
                    xt = xts[ti % 2]
                    kb.dma(sp, (lambda xt=xt, ti=ti: nc.sync.dma_start(out=xt[:], in_=h2_d[ti * 128:(ti + 1) * 128, :])),
                           r=[trk_h2[ti]], w=[xt])
                    xpT = xpTs[ti % 2]
                    nt.run(xt, 128, "ffn_norm_g", xpT, 0)
                    xnb = xpT
                    for c in range(16):
                        ps = psA[(c // 4) % 2]
                        for dc in range(8):
                            kb.op(pe, (lambda dc=dc, c=c, ps=ps: nc.tensor.matmul(
                                ps[:, (c % 4) * 128:(c % 4 + 1) * 128], lhsT=Wpq[:, dc, c * 128:(c + 1) * 128], rhs=xpT[:, dc, :],
                                start=(dc == 0), stop=(dc == 7))), r=[Wpq, xpT], w=[ps])
                        if c % 4 == 3:
                            kb.op(act, (lambda c=c, ps=ps: nc.scalar.copy(
                                out=qpT[:, c - 3:c + 1, :], in_=AP(ps[:], 0, [[512, 128], [128, 4], [1, 128]]))), r=[ps], w=[qpT.k(c // 4)])
                    for cg in range(4):
                        ps = psA[cg % 2]
                        for c in range(cg * 4, cg * 4 + 4):
                            kb.op(pe, (lambda c=c, ps=ps: nc.tensor.matmul(
                                ps[:, (c % 4) * 128:(c % 4 + 1) * 128], lhsT=qpT[:, c, :], rhs=keysT[:, c, :], start=True, stop=True)),
                                r=[qpT.k(c // 4), keysT], w=[ps])
                        kb.op(act, (lambda cg=cg, ps=ps: nc.scalar.copy(
                            out=s_sb[:, cg * 4:cg * 4 + 4, :], in_=AP(ps[:], 0, [[512, 128], [128, 4], [1, 128]]))), r=[ps], w=[s_sb.k(cg)])
                    for c in range(16):
                        kb.op(dve, (lambda c=c: nc.vector.max(out=v16[:, c, 0:8], in_=s_sb[:, c, :])), r=[s_sb.k(c // 4)], w=[v16])
                        kb.op(dve, (lambda c=c: nc.vector.max_index(out=ix16[:, c, 0:8], in_max=v16[:, c, 0:8], in_values=s_sb[:, c, :])),
                              r=[s_sb.k(c // 4), v16], w=[ix16])
                        kb.op(dve, (lambda c=c: nc.vector.match_replace(out=s2_sb[:, c, :], in_to_replace=v16[:, c, 0:8],
                                                                        in_values=s_sb[:, c, :], imm_value=-1e30)),
                              r=[s_sb.k(c // 4), v16], w=[s2_sb])
                        kb.op(dve, (lambda c=c: nc.vector.max(out=v16[:, c, 8:16], in_=s2_sb[:, c, :])), r=[s2_sb], w=[v16])
                        kb.op(dve, (lambda c=c: nc.vector.max_index(out=ix16[:, c, 8:16], in_max=v16[:, c, 8:16], in_values=s2_sb[:, c, :])),
                              r=[s2_sb, v16], w=[ix16])
                    for h in range(8):
                        kb.op(dve, (lambda h=h: nc.vector.tensor_tensor(
                            out=AP(cand[:], h * 256, [[2048, 128], [16, 16], [1, 16]]),
                            in0=AP(v16[:], h * 32, [[256, 128], [1, 16], [0, 16]]),
                            in1=AP(v16[:], h * 32 + 16, [[256, 128], [0, 16], [1, 16]]), op=ALU.add)), r=[v16], w=[cand])
                    for h in range(8):
                        kb.op(dve, (lambda h=h: nc.vector.max(out=sc[:, h, 0:8], in_=cand[:, h, :])), r=[cand], w=[sc])
                        kb.op(dve, (lambda h=h: nc.vector.max_index(out=ci[:, h, 0:8], in_max=sc[:, h, 0:8], in_values=cand[:, h, :])),
                              r=[cand, sc], w=[ci])
                        kb.op(dve, (lambda h=h: nc.vector.match_replace(out=cand2[:, h, :], in_to_replace=sc[:, h, 0:8],
                                                                        in_values=cand[:, h, :], imm_value=-1e30)),
                              r=[cand, sc], w=[cand2])
                        kb.op(dve, (lambda h=h: nc.vector.max(out=sc[:, h, 8:16], in_=cand2[:, h, :])), r=[cand2], w=[sc])
                        kb.op(dve, (lambda h=h: nc.vector.max_index(out=ci[:, h, 8:16], in_max=sc[:, h, 8:16], in_values=cand2[:, h, :])),
                              r=[cand2, sc], w=[ci])
                    civ = AP(ci[:], 0, [[128, 128], [1, 128]])
                    kb.op(dve, lambda: nc.vector.tensor_single_scalar(out=abu[:, 0, :], in_=civ, scalar=4, op=ALU.logical_shift_right),
                          r=[ci], w=[abu])
                    kb.op(dve, lambda: nc.vector.tensor_single_scalar(out=abu[:, 1, :], in_=civ, scalar=15, op=ALU.bitwise_and),
                          r=[ci], w=[abu])
                    kb.op(dve, lambda: nc.vector.tensor_copy(out=abf[:], in_=abu[:]), r=[abu], w=[abf])
                    for hf in range(2):
                        kb.op(dve, (lambda hf=hf: nc.vector.tensor_copy(
                            out=AP(i12f[:], hf * 128, [[256, 128], [16, 8], [1, 16]]),
                            in_=AP(ix16[:], hf * 16, [[256, 128], [32, 8], [1, 16]]))), r=[ix16], w=[i12f])
                    for hf in range(2):
                        for h in range(8):
                            kb.op(dve, (lambda hf=hf, h=h: nc.vector.tensor_tensor(
                                out=eq[:, h, :, :], in0=AP(abf[:], hf * 128 + h * 16, [[256, 128], [1, 16], [0, 16]]),
                                in1=AP(iota[:], 0, [[16, 128], [0, 16], [1, 16]]), op=ALU.is_equal)), r=[abf, iota], w=[eq])
                            kb.op(dve, (lambda hf=hf, h=h: nc.vector.tensor_tensor(
                                out=eq[:, h, :, :], in0=eq[:, h, :, :],
                                in1=AP(i12f[:], hf * 128 + h * 16, [[256, 128], [0, 16], [1, 16]]), op=ALU.mult)), r=[eq, i12f], w=[eq])
                        for wd in (8, 4, 2, 1):
                            kb.op(dve, (lambda wd=wd: nc.vector.tensor_tensor(
                                out=AP(eq[:], 0, [[2048, 128], [16, 128], [1, wd]]), in0=AP(eq[:], 0, [[2048, 128], [16, 128], [1, wd]]),
                                in1=AP(eq[:], wd, [[2048, 128], [16, 128], [1, wd]]), op=ALU.add)), r=[eq], w=[eq])
                        kb.op(dve, (lambda hf=hf: nc.vector.tensor_copy(
                            out=i12b[:, hf, :], in_=AP(eq[:], 0, [[2048, 128], [16, 128]]))), r=[eq], w=[i12b])
                    for hf in range(2):
                        kb.op(pe, (lambda hf=hf: nc.tensor.transpose(out=psTb[:, hf * 128:(hf + 1) * 128], in_=i12b[:, hf, :],
                                                                     identity=identb[:])), r=[i12b, identb], w=[psTb])
                    kb.op(act, lambda: nc.scalar.copy(out=AP(i12T[:], 0, [[256, 128], [1, 256]]), in_=psTb[:, 0:256]), r=[psTb], w=[i12T])
                    kb.op(dve, lambda: nc.vector.scalar_tensor_tensor(out=eTf[:], in0=i12T[:, 0, :], scalar=128.0, in1=i12T[:, 1, :],
                                                                      op0=ALU.mult, op1=ALU.add), r=[i12T], w=[eTf])
                    eT = eTi[ti % 2]
                    kb.op(dve, (lambda eT=eT: nc.vector.tensor_copy(out=eT[:], in_=eTf[:])), r=[eTf], w=[eT])
                    kb.op(dve, lambda: nc.vector.tensor_tensor(out=dsc[:], in0=sc[:], in1=AP(sc[:], 0, [[128, 128], [16, 8], [0, 16]]),
                                                               op=ALU.subtract), r=[sc], w=[dsc])
                    for h in range(8):
                        kb.op(act, (lambda h=h: nc.scalar.activation(out=ex[:, h, :], in_=dsc[:, h, :], func=AF.Exp,
                                                                     accum_out=zsum[:, h:h + 1])), r=[dsc], w=[ex, zsum])
                    kb.op(dve, lambda: nc.vector.reciprocal(out=zsum[:], in_=zsum[:]), r=[zsum], w=[zsum])
                    kb.op(dve, lambda: nc.vector.tensor_tensor(out=gw[:], in0=ex[:], in1=AP(zsum[:], 0, [[8, 128], [1, 8], [0, 16]]),
                                                               op=ALU.mult), r=[ex, zsum], w=[gw])
                    psg = psA[0]
                    kb.op(pe, lambda: nc.tensor.transpose(out=psg[:, 0:128], in_=AP(gw[:], 0, [[128, 128], [1, 128]]), identity=identf[:]),
                          r=[gw, identf], w=[psg])
                    gT = gwT[ti % 2]
                    kb.op(act, (lambda gT=gT: nc.scalar.copy(out=gT[:], in_=psg[:, 0:128])), r=[psg], w=[gT])
                    return xt, xnb, eT, gT

                def tokens(ti, xt, xnb, eT, gT, th):
                    gi = gi_box[0]
                    def finalize(g8, toks, gT=gT):
                        cTg = cT[g8 % 2]
                        ZG = zg[g8 % 2]
                        kb.op(act, lambda: nc.scalar.activation(out=agl[:], in_=psAd[:, g8 * 8:(g8 + 1) * 8], func=AF.Gelu),
                              r=[psAd.k(g8)], w=[agl])
                        kb.op(dve, (lambda: nc.vector.tensor_tensor(
                            out=cTg[:], in0=agl[:], in1=gT[:, g8 * 8:(g8 + 1) * 8], op=ALU.mult)), r=[agl, gT], w=[cTg])
                        kb.op(dve, (lambda: nc.vector.tensor_tensor(
                            out=ZG[:], in0=wband[:, :, 120 - 8 * g8:248 - 8 * g8], in1=AP(cTg[:], 0, [[8, 128], [1, 8], [0, 128]]),
                            op=ALU.mult)), r=[wband, cTg], w=[ZG])
                        return ZG

                    def vmm(g8, toks, ZG, j):
                        t = g8 * 8 + j
                        v_ = toks[j]
                        for half in range(2):
                            kb.op(pe, (lambda v_=v_, half=half, t=t, j=j: nc.tensor.matmul(
                                psY[:, half * 512:(half + 1) * 512], lhsT=ZG[:, j, :], rhs=v_[:, D + half * 512:D + (half + 1) * 512],
                                start=(t == 0), stop=(t == 127))), r=[ZG, v_], w=[psY])

                    xpT = xnb

                    def dotmm(pm):
                        t_, us_, g_ = pm
                        for dc in range(8):
                            kb.op(pe, (lambda dc=dc, t_=t_, us_=us_: nc.tensor.matmul(
                                psAd[:, t_:t_ + 1], lhsT=us_[:, dc, :], rhs=xpT[:, dc, t_:t_ + 1], start=(dc == 0), stop=(dc == 7))),
                                r=[us_, xpT], w=[psAd.k(g_)])

                    prev = None
                    pend = None
                    pZG = None
                    for g8 in range(16):
                        toks = []
                        for j in range(8):
                            t = g8 * 8 + j
                            uv_ = uvr[gi % len(uvr)]
                            toks.append(uv_)
                            gi += 1
                            kb.dma(pool, (lambda uv_=uv_, t=t, eT=eT: nc.gpsimd.indirect_dma_start(
                                out=uv_[:], out_offset=None, in_=uv_d,
                                in_offset=bass.IndirectOffsetOnAxis(ap=eT[:, t:t + 1], axis=0))), r=[eT] + trk_ub + trk_vb, w=[uv_])
                            if j == 0 and pend is not None:
                                dotmm(pend)
                                pend = None
                                pZG = finalize(prev[0], prev[1])
                            pu = psU[t % 2]
                            for dc in range(8):
                                kb.op(pe, (lambda dc=dc, pu=pu, uv_=uv_: nc.tensor.transpose(
                                    out=pu[:, dc * 128:(dc + 1) * 128], in_=uv_[:, dc * 128:(dc + 1) * 128], identity=identb[:])),
                                    r=[uv_, identb], w=[pu])
                            us = UTs[t % 3]
                            kb.op(act, (lambda pu=pu, us=us: nc.scalar.copy(out=AP(us[:], 0, [[D, 128], [1, D]]), in_=pu[:])), r=[pu], w=[us])
                            if pend is not None:
                                dotmm(pend)
                            pend = (t, us, g8)
                            if prev is not None and j >= 2:
                                vmm(prev[0], prev[1], pZG, j - 2)
                            kb.flush(th, 5)
                        if prev is not None:
                            vmm(prev[0], prev[1], pZG, 6)
                            vmm(prev[0], prev[1], pZG, 7)
                        prev = (g8, toks)
                    dotmm(pend)
                    pZG = finalize(prev[0], prev[1])
                    for j in range(8):
                        vmm(prev[0], prev[1], pZG, j)
                    for half in range(2):
                        kb.op(dve, (lambda half=half, xt=xt: nc.vector.tensor_tensor(
                            out=xt[:, half * 512:(half + 1) * 512], in0=psY[:, half * 512:(half + 1) * 512],
                            in1=xt[:, half * 512:(half + 1) * 512], op=ALU.add)), r=[psY, xt], w=[xt])
                    kb.op(act, (lambda xt=xt: nc.scalar.activation(out=nt.junk[0][:], in_=xt[:], func=AF.Square, accum_out=ssf[:])),
                          r=[xt], w=[nt.junk[0], ssf])
                    kb.op(act, lambda: nc.scalar.activation(out=rsf[:], in_=ssf[:], func=AF.Sqrt, bias=epsc[:], scale=1.0 / D),
                          r=[ssf, epsc], w=[rsf])
                    kb.op(dve, lambda: nc.vector.reciprocal(out=rsf[:], in_=rsf[:]), r=[rsf], w=[rsf])
                    kb.op(dve, (lambda xt=xt: nc.vector.scalar_tensor_tensor(out=AP(eq[:], 0, [[2048, 128], [1, D]]), in0=xt[:], scalar=rsf[:], in1=gfin[:],
                                                                             op0=ALU.mult, op1=ALU.mult)), r=[xt, rsf, gfin], w=[eq])
                    kb.dma(sp, (lambda ti=ti: nc.sync.dma_start(out=out[ti * 128:(ti + 1) * 128, :], in_=AP(eq[:], 0, [[2048, 128], [1, D]]))), r=[eq], w=[trk_out])
                    gi_box[0] = gi

                kb.defer = []
                st = prologue(0)
                th = kb.defer
                kb.defer = None
                kb.flush(th)
                for ti in range(16):
                    th = []
                    if ti < 15:
                        kb.defer = []
                        st_next = prologue(ti + 1)
                        th = kb.defer
                        kb.defer = None
                    tokens(ti, st[0], st[1], st[2], st[3], th)
                    kb.flush(th)
                    if ti < 15:
                        st = st_next

        if "p3" in PHASES:
            phase3()

        PHASE_REST(locals())

        kb.finish(trk_kT + trk_v + trk_convo + trk_h1 + trk_h2 + [trk_out] + dbg_trks + trk_ub + trk_vb)
    return nc


PHASES = ("p0", "p1a", "p1b", "p2", "p3")
STOP = 0
NO_SELF_WAIT = ("pe",)
DEBUG = False


class StopBuild(Exception):
    pass


def ck(n):
    if STOP == n:
        raise StopBuild()

DBG_KIND = {}


def PHASE_REST(L):
    pass


def _const_tables(par):
    slopes = 2.0 ** (-np.arange(1, 9, dtype=np.float64))
    abias = np.zeros((16, 128, 8, 16), np.float32)
    vpen = np.zeros((16, 128, 16), np.float32)
    notown = np.ones((16, 128, 16), np.float32)
    p = np.arange(128)
    for c in range(16):
        i, s = c // 2, c % 2
        g = 2 * i + par
        t = g * 256 + s * 128 + p
        for n in range(16):
            if n > g:
                abias[c, :, :, n] = NEG
                vpen[c, :, n] = -1e30
            else:
                abias[c, :, :, n] = -(slopes[None, :] * (t[:, None] - n * 256))
                if n == g:
                    vpen[c, :, n] = -1e30
                    notown[c, :, n] = 0.0
    kl = np.arange(256)
    causal = np.zeros((2, 128, 256), np.float32)
    for s in range(2):
        causal[s] = np.where(kl[None, :] <= (s * 128 + p)[:, None], 0.0, NEG)
    cpa = causal if par == 0 else np.zeros_like(causal)
    cpb = causal
    krow = np.zeros((8, 512), np.float32)
    for h in range(8):
        krow[h] = slopes[h] * (np.arange(512) % 256)
    return dict(abias=abias.reshape(16, 128, 128), vpen=vpen, notown=notown, cpa=cpa, cpb=cpb,
                krow=krow.reshape(1, 8 * 512), identf=np.eye(128, dtype=np.float32),
                iota16=np.tile(np.arange(16, dtype=np.float32), (128, 1)))


def make_in_maps(inputs, cores=range(8)):
    x = np.asarray(inputs["x"], np.float32)
    mem = np.asarray(inputs["mem"], np.float32)
    shared = {}
    for nm in ["mix_norm_g", "w_in", "conv_dw_w", "conv_dw_b", "conv_ln_g", "conv_ln_b", "w_conv_out", "b_conv_out",
               "w_attn_out", "w_mix_out", "xattn_norm_g", "mem_norm_g", "w_xq", "w_xkv", "w_xo", "ffn_norm_g",
               "w_peer_q", "peer_u", "peer_v"]:
        shared[nm] = np.ascontiguousarray(np.asarray(inputs[nm], np.float32)[0])
    shared["peer_subkeys"] = np.ascontiguousarray(np.asarray(inputs["peer_subkeys"], np.float32)[0].reshape(16, 128, 128))
    shared["final_norm_g"] = np.ascontiguousarray(np.asarray(inputs["final_norm_g"], np.float32))
    maps = []
    for c in cores:
        b, par = c // 2, c % 2
        xb = x[b]
        blocks = [2 * i + par for i in range(8)]
        xown = np.concatenate([xb[g * 256:(g + 1) * 256] for g in blocks], axis=0)
        xconv = np.zeros((8, 288, D), np.float32)
        for i, g in enumerate(blocks):
            lo = g * 256 - 32
            if lo >= 0:
                xconv[i] = xb[lo:lo + 288]
            else:
                xconv[i, 32:] = xb[0:256]
        m = dict(shared)
        m.update(_const_tables(par))
        m["xfull"] = np.ascontiguousarray(xb)
        m["xown"] = np.ascontiguousarray(xown)
        m["xconv"] = xconv
        m["mem"] = np.ascontiguousarray(mem[b])
        maps.append(m)
    return maps


def kernel(**inputs):
    nc = build_program()
    maps = make_in_maps(inputs)
    res = run_bass_kernel_spmd(nc, maps, core_ids=list(range(8)))
    out = np.zeros((4, SEQ, D), np.float32)
    for c in range(8):
        b, par = c // 2, c % 2
        o = np.asarray(res.results[c]["out"], np.float32)
        for i in range(8):
            g = 2 * i + par
            out[b, g * 256:(g + 1) * 256] = o[i * 256:(i + 1) * 256]
    return out
```
